# Optimizing a Trainium2 kernel written in Bass

```python
import math
import jax, jax.numpy as jnp
from jax import lax
import numpy as np

D_MODEL = 1024
BATCH = 32
SEQ = 2048
DEPTH = 1

N_DIFF_HEADS = 4
DIFF_HEAD_DIM = 64
DIFF_V_DIM = 2 * DIFF_HEAD_DIM
DIFF_WIDTH = N_DIFF_HEADS * DIFF_V_DIM
N_MLSTM_HEADS = 4
MLSTM_QK_DIM = 64
MLSTM_V_DIM = 128
MLSTM_WIDTH = N_MLSTM_HEADS * MLSTM_V_DIM
MIX_WIDTH = DIFF_WIDTH + MLSTM_WIDTH
CONV_WIDTH = 4
CHUNK = 64
Q_BLOCK = 128
D_FF = 4 * D_MODEL
ROPE_THETA = 10000.0
EPS = 1e-6

IN_SIZES = (
    N_DIFF_HEADS * 2 * DIFF_HEAD_DIM,
    N_DIFF_HEADS * 2 * DIFF_HEAD_DIM,
    DIFF_WIDTH,
    N_MLSTM_HEADS * MLSTM_QK_DIM,
    N_MLSTM_HEADS * MLSTM_QK_DIM,
    MLSTM_WIDTH,
    MLSTM_WIDTH,
    N_MLSTM_HEADS,
    N_MLSTM_HEADS,
)
IN_WIDTH = sum(IN_SIZES)
IN_OFFSETS = tuple(int(o) for o in np.cumsum(IN_SIZES)[:-1])
CONV_CH = 2 * N_MLSTM_HEADS * MLSTM_QK_DIM

kernel_name = "hybrid_diffattn_mlstm_parallel_heads"


def lambda_init(layer):
    return 0.8 - 0.6 * math.exp(-0.3 * layer)


def rmsnorm(x, g):
    xf = x.astype(jnp.float32)
    y = xf * lax.rsqrt(jnp.mean(xf * xf, axis=-1, keepdims=True) + EPS)
    return (y * g.astype(jnp.float32)).astype(x.dtype)


def rope(x, pos):
    d = x.shape[-1]
    inv = ROPE_THETA ** (-jnp.arange(0, d, 2, dtype=jnp.float32) / d)
    ang = pos.astype(jnp.float32)[:, None] * inv[None, :]
    cos = jnp.cos(ang)[None, :, None, :]
    sin = jnp.sin(ang)[None, :, None, :]
    xf = x.astype(jnp.float32)
    x1, x2 = xf[..., : d // 2], xf[..., d // 2:]
    out = jnp.concatenate([x1 * cos - x2 * sin, x2 * cos + x1 * sin], axis=-1)
    return out.astype(x.dtype)


def diff_attention(q, k, v, lam):
    S = q.shape[3]
    scale = DIFF_HEAD_DIM ** -0.5
    outs = []
    for blk in range(S // Q_BLOCK):
        lo, hi = blk * Q_BLOCK, (blk + 1) * Q_BLOCK
        qb = q[:, :, :, lo:hi]
        kb = k[:, :, :, :hi]
        s = jnp.einsum('bhcqd,bhckd->bhcqk', qb, kb).astype(jnp.float32) * scale
        mask = (lo + jnp.arange(Q_BLOCK))[:, None] >= jnp.arange(hi)[None, :]
        s = jnp.where(mask, s, -jnp.inf)
        p = jax.nn.softmax(s, axis=-1)
        p_diff = p[:, :, 0] - lam * p[:, :, 1]
        outs.append(jnp.einsum('bhqk,bhkv->bhqv', p_diff.astype(v.dtype), v[:, :, :hi]))
    return jnp.concatenate(outs, axis=2)


def mlstm_chunkwise(q, k, v, i_pre, logf):
    B, H, S, dqk = q.shape
    dv = v.shape[-1]
    nc = S // CHUNK
    q = q * (dqk ** -0.5)

    def chunked(t):
        t = t.reshape(t.shape[:2] + (nc, CHUNK) + t.shape[3:])
        return jnp.moveaxis(t, 2, 0)

    xs = (chunked(q), chunked(k), chunked(v), chunked(i_pre), chunked(logf))
    causal = jnp.tril(jnp.ones((CHUNK, CHUNK), dtype=bool))

    def step(carry, inp):
        C, n, m = carry
        qc, kc, vc, ic, fc = inp
        b = jnp.cumsum(fc, axis=-1)
        g = b[..., -1]
        logD = b[..., :, None] - b[..., None, :] + ic[..., None, :]
        logD = jnp.where(causal, logD, -jnp.inf)
        inter = b + m[..., None]
        m_comb = jnp.maximum(inter, jnp.max(logD, axis=-1))
        scores = jnp.einsum('bhjd,bhsd->bhjs', qc, kc) * jnp.exp(logD - m_comb[..., None])
        w_inter = jnp.exp(inter - m_comb)
        num = (w_inter[..., None] * jnp.einsum('bhjd,bhdv->bhjv', qc, C)
               + jnp.einsum('bhjs,bhsv->bhjv', scores, vc))
        den = w_inter * jnp.einsum('bhjd,bhd->bhj', qc, n) + jnp.sum(scores, axis=-1)
        h = num / jnp.maximum(jnp.abs(den), jnp.exp(-m_comb))[..., None]
        log_w = g[..., None] - b + ic
        m_new = jnp.maximum(g + m, jnp.max(log_w, axis=-1))
        decay = jnp.exp(g + m - m_new)
        w = jnp.exp(log_w - m_new[..., None])
        C_new = decay[..., None, None] * C + jnp.einsum('bhs,bhsd,bhsv->bhdv', w, kc, vc)
        n_new = decay[..., None] * n + jnp.einsum('bhs,bhsd->bhd', w, kc)
        return (C_new, n_new, m_new), h

    init = (jnp.zeros((B, H, dqk, dv), jnp.float32),
            jnp.zeros((B, H, dqk), jnp.float32),
            jnp.zeros((B, H), jnp.float32))
    _, hs = lax.scan(step, init, xs)
    return jnp.moveaxis(hs, 0, 2).reshape(B, H, S, dv)


def causal_dwconv(x, w, b):
    S = x.shape[1]
    xp = jnp.pad(x, ((0, 0), (CONV_WIDTH - 1, 0), (0, 0)))
    y = b
    for j in range(CONV_WIDTH):
        y = y + w[j] * xp[:, j:j + S]
    return y


def setup_inputs(seed: int = 0) -> dict:
    key = jax.random.key(seed)
    ks = jax.random.split(key, 20)
    f32 = jnp.float32
    nrm = lambda k, shape, s: jax.random.normal(k, shape, f32) * s
    gain = lambda k, shape: 1.0 + 0.05 * jax.random.normal(k, shape, f32)
    return {
        "x": jax.random.normal(ks[0], (BATCH, SEQ, D_MODEL), f32),
        "norm_mix_pre": gain(ks[1], (DEPTH, D_MODEL)),
        "w_in": nrm(ks[2], (DEPTH, D_MODEL, IN_WIDTH), D_MODEL ** -0.5),
        "conv_w": nrm(ks[3], (DEPTH, CONV_WIDTH, CONV_CH), CONV_WIDTH ** -0.5),
        "conv_b": nrm(ks[4], (DEPTH, CONV_CH), 0.01),
        "b_igate": nrm(ks[5], (DEPTH, N_MLSTM_HEADS), 0.1),
        "b_fgate": 3.0 + nrm(ks[6], (DEPTH, N_MLSTM_HEADS), 0.5),
        "lambda_q1": nrm(ks[7], (DEPTH, DIFF_HEAD_DIM), 0.1),
        "lambda_k1": nrm(ks[8], (DEPTH, DIFF_HEAD_DIM), 0.1),
        "lambda_q2": nrm(ks[9], (DEPTH, DIFF_HEAD_DIM), 0.1),
        "lambda_k2": nrm(ks[10], (DEPTH, DIFF_HEAD_DIM), 0.1),
        "diff_norm": gain(ks[11], (DEPTH, DIFF_V_DIM)),
        "mlstm_norm": gain(ks[12], (DEPTH, N_MLSTM_HEADS, MLSTM_V_DIM)),
        "w_out": nrm(ks[13], (DEPTH, MIX_WIDTH, D_MODEL), MIX_WIDTH ** -0.5),
        "norm_mix_post": gain(ks[14], (DEPTH, D_MODEL)),
        "norm_mlp_pre": gain(ks[15], (DEPTH, D_MODEL)),
        "w_up": nrm(ks[16], (DEPTH, D_MODEL, D_FF), D_MODEL ** -0.5),
        "w_down": nrm(ks[17], (DEPTH, D_FF, D_MODEL), D_FF ** -0.5),
        "norm_mlp_post": gain(ks[18], (DEPTH, D_MODEL)),
    }


def reference(x, norm_mix_pre, w_in, conv_w, conv_b, b_igate, b_fgate,
              lambda_q1, lambda_k1, lambda_q2, lambda_k2, diff_norm, mlstm_norm,
              w_out, norm_mix_post, norm_mlp_pre, w_up, w_down, norm_mlp_post):
    B, S, _ = x.shape
    pos = jnp.arange(S, dtype=jnp.int32)
    for l in range(DEPTH):
        lam_init = lambda_init(l)
        h = rmsnorm(x, norm_mix_pre[l])
        proj = h @ w_in[l]
        dq, dk, dvv, mq, mk, mv, mo, mi, mf = jnp.split(proj, IN_OFFSETS, axis=-1)

        dq = rope(dq.reshape(B, S, 2 * N_DIFF_HEADS, DIFF_HEAD_DIM), pos)
        dk = rope(dk.reshape(B, S, 2 * N_DIFF_HEADS, DIFF_HEAD_DIM), pos)
        dq = dq.reshape(B, S, N_DIFF_HEADS, 2, DIFF_HEAD_DIM).transpose(0, 2, 3, 1, 4)
        dk = dk.reshape(B, S, N_DIFF_HEADS, 2, DIFF_HEAD_DIM).transpose(0, 2, 3, 1, 4)
        dvv = dvv.reshape(B, S, N_DIFF_HEADS, DIFF_V_DIM).transpose(0, 2, 1, 3)
        lam = (jnp.exp(jnp.sum(lambda_q1[l].astype(jnp.float32) * lambda_k1[l].astype(jnp.float32)))
               - jnp.exp(jnp.sum(lambda_q2[l].astype(jnp.float32) * lambda_k2[l].astype(jnp.float32)))
               + lam_init)
        o_diff = diff_attention(dq, dk, dvv, lam)
        o_diff = rmsnorm(o_diff, diff_norm[l]) * (1.0 - lam_init)
        o_diff = o_diff.transpose(0, 2, 1, 3).reshape(B, S, DIFF_WIDTH)

        qk = causal_dwconv(jnp.concatenate([mq, mk], axis=-1), conv_w[l], conv_b[l])
        qk = jax.nn.silu(qk)
        mq, mk = qk[..., :CONV_CH // 2], qk[..., CONV_CH // 2:]
        to_heads = lambda t, d: t.reshape(B, S, N_MLSTM_HEADS, d).transpose(0, 2, 1, 3).astype(jnp.float32)
        i_pre = (mi + b_igate[l]).astype(jnp.float32).transpose(0, 2, 1)
        logf = jax.nn.log_sigmoid((mf + b_fgate[l]).astype(jnp.float32)).transpose(0, 2, 1)
        hm = mlstm_chunkwise(to_heads(mq, MLSTM_QK_DIM), to_heads(mk, MLSTM_QK_DIM),
                             to_heads(mv, MLSTM_V_DIM), i_pre, logf)
        hm = rmsnorm(hm.transpose(0, 2, 1, 3), mlstm_norm[l]).astype(x.dtype)
        o_mlstm = hm.reshape(B, S, MLSTM_WIDTH) * jax.nn.sigmoid(mo)

        mixed = jnp.concatenate([o_diff.astype(x.dtype), o_mlstm], axis=-1) @ w_out[l]
        x = x + rmsnorm(mixed, norm_mix_post[l])

        h = rmsnorm(x, norm_mlp_pre[l])
        u = jnp.square(jax.nn.relu(h @ w_up[l]))
        x = x + rmsnorm(u @ w_down[l], norm_mlp_post[l])
    return x
```

```python
import math
import numpy as np
import ml_dtypes
import concourse.bass as bass
import concourse.mybir as mybir
from concourse.bass_utils import run_bass_kernel_spmd

F32 = mybir.dt.float32
BF16 = mybir.dt.bfloat16
AF = mybir.ActivationFunctionType
ALU = mybir.AluOpType
AX = mybir.AxisListType

S = 2048
D = 1024
CH = 512
NCH = S // CH
DFF = 4096
EPS = 1e-6
LAM_INIT = 0.8 - 0.6 * math.exp(-0.3 * 0)
NW = 4
DBG_STAGE = None
ROPE_ADD_ENG = "pool"


class _Stop(Exception):
    pass


def _stage(n):
    if DBG_STAGE is not None and DBG_STAGE == n:
        raise _Stop()


class Trk:
    __slots__ = ("w", "r", "dcnt", "name", "sem", "excl")

    def __init__(self, name):
        self.excl = False
        self.w = None
        self.r = {}
        self.dcnt = 0
        self.name = name
        self.sem = None


class Buf:
    def __init__(self, t, name, nsub=1):
        self.t = t
        self.name = name
        self.trk = [Trk(f"{name}.{i}") for i in range(nsub)]

    def __getitem__(self, i):
        return self.trk[i]


def _trks(lst):
    out = []
    for x in lst:
        if isinstance(x, Buf):
            out.extend(x.trk)
        elif isinstance(x, Trk):
            out.append(x)
        else:
            out.extend(_trks(x))
    return out


class Prog:
    ENG = ["pe", "act", "dve", "pool", "sp"]

    def __init__(self, nc):
        self.nc = nc
        self.ops = {e: [] for e in self.ENG}
        self.owners = []
        self.stores = []

    def _deps(self, eng, r, w):
        deps = {}

        def add(tok):
            key, val = tok
            if key[0] == "E" and key[1] == "pe" and eng == "pe":
                return
            if deps.get(key, -1) < val:
                deps[key] = val

        for t in r:
            if t.w is not None:
                add(t.w)
            if t.excl:
                for k, v in t.r.items():
                    if k != ("E", eng):
                        add((k, v))
        for t in w:
            if t.w is not None:
                add(t.w)
            for k, v in t.r.items():
                add((k, v))
        return deps

    def _commit(self, tok, r, w):
        key, val = tok
        for t in r:
            if t.r.get(key, -1) < val:
                t.r[key] = val
        for t in w:
            t.w = tok
            t.r = {}

    def op(self, eng, fn, r=(), w=()):
        r = _trks(r)
        w = _trks(w)
        deps = self._deps(eng, r, w)
        idx = len(self.ops[eng])
        tok = (("E", eng), idx)
        self.ops[eng].append(dict(fn=fn, deps=deps, kind="E", idx=idx))
        self._commit(tok, r, w)

    def dma(self, eng, fn, owner, r=(), w=(), store=False):
        r = _trks(r)
        w = _trks(w)
        deps = self._deps(eng, r, w)
        if owner.sem is None:
            owner.sem = "pending"
            self.owners.append(owner)
        key = ("D", id(owner))
        if owner.dcnt > 0:
            if deps.get(key, -1) < owner.dcnt:
                deps[key] = owner.dcnt
        owner.dcnt += 1
        tok = (key, owner.dcnt)
        self.ops[eng].append(dict(fn=fn, deps=deps, kind="D", owner=owner))
        self._commit(tok, r, w)
        if store:
            self.stores.append(tok)

    def emit(self, stack):
        nc = self.nc
        sig = {e: set() for e in self.ENG}
        for e in self.ENG:
            for o in self.ops[e]:
                for key, val in o["deps"].items():
                    if key[0] == "E":
                        sig[key[1]].add(val)
        for tok in self.stores:
            pass
        rank = {}
        for e in self.ENG:
            rank[e] = {idx: i + 1 for i, idx in enumerate(sorted(sig[e]))}
        esem = {e: stack.enter_context(nc.semaphore(f"sem_{e}")) for e in self.ENG}
        osem = {}
        for i, o in enumerate(self.owners):
            osem[id(o)] = stack.enter_context(nc.semaphore(f"dsem_{i}"))
        block = stack.enter_context(nc.Block())
        hooks = dict(pe=block.tensor, act=block.scalar, dve=block.vector, pool=block.gpsimd, sp=block.sync)
        stores = self.stores

        def make(e):
            def body(engine):
                waited = {}
                for o in self.ops[e]:
                    for key, val in o["deps"].items():
                        if key[0] == "E":
                            sem = esem[key[1]]
                            v = rank[key[1]][val]
                        else:
                            sem = osem[key[1]]
                            v = 16 * val
                        if waited.get(key, 0) < v:
                            engine.wait_ge(sem, v)
                            waited[key] = v
                    inst = o["fn"](engine)
                    if o["kind"] == "E":
                        if o["idx"] in rank[e]:
                            inst.then_inc(esem[e], 1)
                    else:
                        inst.then_inc(osem[id(o["owner"])], 16)
                if e == "sp":
                    fin = {}
                    for key, val in stores:
                        fin[key] = max(fin.get(key, 0), val)
                    for key, val in fin.items():
                        if waited.get(key, 0) < 16 * val:
                            engine.wait_ge(osem[key[1]], 16 * val)
            return body

        for e in self.ENG:
            hooks[e](make(e))


def _consts():
    bf = ml_dtypes.bfloat16
    c = {}
    eye = np.eye(128, dtype=np.float32)
    c["ident_bf"] = eye.astype(bf)
    c["ident_f"] = eye.copy()
    perm = np.zeros((128, 128), np.float32)
    for k in range(128):
        m = (k // 64) * 64 + ((k % 64) + 32) % 64
        perm[k, m] = 1.0
    c["perm_bf"] = perm.astype(bf)
    c["ones_bf"] = np.ones((128, 128), bf)
    c["ones_f"] = np.ones((128, 128), np.float32)
    kk = np.arange(128)[:, None]
    jj = np.arange(128)[None, :]
    triu = (kk <= jj).astype(np.float32)
    c["triu_f"] = triu
    c["mask_bf"] = triu.astype(bf)
    c["maskneg4"] = np.tile(np.where(kk > jj, -30000.0, 0.0).astype(np.float32), (1, 4))
    p = np.arange(128)
    inv = 10000.0 ** (-np.arange(0, 64, 2, dtype=np.float64) / 64.0)
    ang = np.arange(S, dtype=np.float64)[None, :] * inv[p % 32][:, None]
    sign = np.where((p % 64) < 32, -1.0, 1.0)[:, None]
    c["cos_t"] = np.cos(ang).astype(np.float32)
    c["sin_t"] = (sign * np.sin(ang)).astype(np.float32)
    return c


CONST_SPECS = [
    ("ident_bf", [128, 128], BF16), ("ident_f", [128, 128], F32), ("perm_bf", [128, 128], BF16),
    ("ones_bf", [128, 128], BF16), ("ones_f", [128, 128], F32), ("triu_f", [128, 128], F32),
    ("mask_bf", [128, 128], BF16), ("maskneg4", [128, 512], F32),
]
PARAM_SPECS = [
    ("gpre", [128, 8]), ("gmlp", [128, 8]), ("gpost_b", [128, 1024]), ("gmlppost_b", [128, 1024]),
    ("convw", [128, 16]), ("convb", [128, 4]), ("gbias", [128, 8]), ("lamv", [128, 256]),
    ("gdiff", [128, 1]), ("gml", [128, 4]),
]


def _params(inp):
    f = lambda a: np.ascontiguousarray(np.asarray(a, dtype=np.float32))
    p = {}
    p["gpre"] = f(inp["norm_mix_pre"][0].reshape(8, 128).T)
    p["gmlp"] = f(inp["norm_mlp_pre"][0].reshape(8, 128).T)
    p["gpost_b"] = f(np.broadcast_to(inp["norm_mix_post"][0][None, :], (128, 1024)))
    p["gmlppost_b"] = f(np.broadcast_to(inp["norm_mlp_post"][0][None, :], (128, 1024)))
    cw = np.asarray(inp["conv_w"][0])
    p["convw"] = f(cw.T.reshape(4, 128, 4).transpose(1, 0, 2).reshape(128, 16))
    p["convb"] = f(np.asarray(inp["conv_b"][0]).reshape(4, 128).T)
    gb = np.concatenate([np.asarray(inp["b_igate"][0]), np.asarray(inp["b_fgate"][0])])
    p["gbias"] = f(np.broadcast_to(gb[None, :], (128, 8)))
    lv = np.concatenate([np.asarray(inp[k][0]) for k in ("lambda_q1", "lambda_k1", "lambda_q2", "lambda_k2")])
    p["lamv"] = f(np.broadcast_to(lv[None, :], (128, 256)))
    p["gdiff"] = f(np.asarray(inp["diff_norm"][0])[:, None])
    p["gml"] = f(np.asarray(inp["mlstm_norm"][0]).T)
    return p


def build(nseq):
    from contextlib import ExitStack

    nc = bass.Bass("TRN2", target_bir_lowering=False)
    NT = nseq * S
    dram = {}
    dram["x"] = nc.dram_tensor("x", [NT, D], F32, kind="ExternalInput")
    dram["out"] = nc.dram_tensor("out", [NT, D], F32, kind="ExternalOutput")
    dram["w_in"] = nc.dram_tensor("w_in", [D, 3080], F32, kind="ExternalInput")
    dram["w_out"] = nc.dram_tensor("w_out", [D, D], F32, kind="ExternalInput")
    dram["w_up"] = nc.dram_tensor("w_up", [D, DFF], F32, kind="ExternalInput")
    dram["w_down"] = nc.dram_tensor("w_down", [DFF, D], F32, kind="ExternalInput")
    for name, shp, dt in CONST_SPECS:
        dram[name] = nc.dram_tensor(name, shp, dt, kind="ExternalInput")
    dram["cos_t"] = nc.dram_tensor("cos_t", [128, S], F32, kind="ExternalInput")
    dram["sin_t"] = nc.dram_tensor("sin_t", [128, S], F32, kind="ExternalInput")
    for name, shp in PARAM_SPECS:
        dram[name] = nc.dram_tensor(name, shp, F32, kind="ExternalInput")
    dram["wbf"] = nc.dram_tensor("wbf", [24, 128, 8, 512], BF16, kind="Internal")
    wbf_trk = [Trk(f"wbf{i}") for i in range(24)]

    with ExitStack() as st:
        P = Prog(nc)

        def sb(name, shape, dt, nsub=1):
            return Buf(st.enter_context(nc.sbuf_tensor("sb_" + name, shape, dt)), name, nsub)

        def psb(name, shape, dt):
            return Buf(st.enter_context(nc.psum_tensor(name, shape, dt)), name, 1)

        C = {name: sb(name, shp, dt) for name, shp, dt in CONST_SPECS}
        PR = {name: sb(name, shp, F32) for name, shp in PARAM_SPECS}
        const_owner = Trk("const_owner")
        for name in list(C) + list(PR):
            b = C.get(name) or PR[name]
            P.dma("sp", (lambda e, b=b, name=name: e.dma_start(out=b.t[:], in_=dram[name].ap())), const_owner, w=[b])

        kT = sb("kT", [128, 4, S], BF16, nsub=16)
        vd = sb("vd", [128, 16, 512], BF16, nsub=16)
        cs = sb("cs", [128, 2, 512], F32)
        xc = sb("xc", [128, 4, 1024], F32, nsub=4)
        hT = sb("hT", [128, 8, 512], BF16, nsub=4)
        qT = sb("qT", [128, 4, 512], BF16, nsub=4)
        mqT = sb("mqT", [128, 2, 512], BF16, nsub=2)
        mkT = sb("mkT", [128, 2, 512], BF16, nsub=2)
        hal = sb("hal", [128, 4, 4], F32, nsub=4)
        mvb = sb("mvb", [128, 4, 4, 256], BF16, nsub=4)
        ktil = sb("ktil", [128, 256], BF16)
        oT = sb("oT", [128, 8, 512], BF16, nsub=8)
        uTb = [sb("uT0", [128, 8, 512], BF16, nsub=8), sb("uT1", [128, 8, 512], BF16, nsub=8)]
        y = sb("y", [128, 4, 1024], F32, nsub=4)
        wr = [sb(f"wr{i}", [128, 8, 512], BF16) for i in range(NW)]
        wg = sb("wg", [128, 8, 8], BF16)
        wgs = sb("wgs", [128, 8, 8], F32)
        ET = [sb(f"et{i}", [128, 512], BF16) for i in range(4)]
        F = [sb(f"f{i}", [128, 520], F32) for i in range(8)]
        hmS = sb("hmS", [128, 4, 512], F32, nsub=4)
        stt = sb("stt", [128, 4, 256], F32, nsub=4)
        Cbf = sb("Cbf", [128, 4, 256], BF16, nsub=4)
        qsb = sb("qsb", [128, 4, 128], BF16, nsub=4)
        AT = sb("AT", [128, 4, 128], BF16, nsub=4)
        hb = [sb(f"hb{i}", [128, 1024], BF16) for i in range(3)]
        gate = sb("gate", [128, 4, 8], F32, nsub=4)
        lf = sb("lf", [128, 4, 4], F32, nsub=4)
        sm = sb("sm", [128, 64], F32)
        stat = [sb(f"stat{i}", [128, 8], F32) for i in range(4)]
        lam = sb("lam", [128, 4], F32)
        PS = [psb(f"ps{i}", [128, 512], F32) for i in range(8)]
        PSbf = [Buf(PS[i].t.bitcast(BF16) if hasattr(PS[i].t, "bitcast") else None, f"psbf{i}") for i in range(8)]
        for i in range(8):
            PS[i].trk[0].excl = True
            PSbf[i].trk = PS[i].trk

        ring_state = {"a": 0, "hb": 0, "f": 0}

        def ringA():
            i = ring_state["a"]
            ring_state["a"] = (i + 1) % 4
            return i

        def act(out, in_, func, r, w, bias=None, scale=None, accum_out=None):
            kw = {}
            if bias is not None:
                kw["bias"] = bias
            if scale is not None:
                kw["scale"] = scale
            if accum_out is not None:
                kw["accum_out"] = accum_out
            P.op("act", lambda e: e.activation(out=out, in_=in_, func=func, **kw), r=r, w=w)

        def mm(out, lhsT, rhs, start, stop, r, w):
            P.op("pe", lambda e: e.matmul(out, lhsT=lhsT, rhs=rhs, start=start, stop=stop), r=r, w=w)

        def tt(eng, out, in0, in1, op, r, w):
            P.op(eng, lambda e: e.tensor_tensor(out=out, in0=in0, in1=in1, op=op), r=r, w=w)

        def ts(eng, out, in0, s1, op0, r, w, s2=None, op1=None):
            if op1 is None:
                P.op(eng, lambda e: e.tensor_scalar(out=out, in0=in0, scalar1=s1, scalar2=None, op0=op0), r=r, w=w)
            else:
                P.op(eng, lambda e: e.tensor_scalar(out=out, in0=in0, scalar1=s1, scalar2=s2, op0=op0, op1=op1), r=r, w=w)

        def stt_(out, in0, scalar, in1, op0, op1, r, w):
            P.op("dve", lambda e: e.scalar_tensor_tensor(out=out, in0=in0, scalar=scalar, in1=in1, op0=op0, op1=op1), r=r, w=w)

        def cp(eng, out, in_, r, w):
            if eng == "act":
                P.op("act", lambda e: e.activation(out=out, in_=in_, func=AF.Copy), r=r, w=w)
            else:
                P.op(eng, lambda e: e.tensor_copy(out=out, in_=in_), r=r, w=w)

        lv = PR["lamv"]
        tt("dve", F[0].t[:, 0:64], lv.t[:, 0:64], lv.t[:, 64:128], ALU.mult, r=[lv], w=[F[0]])
        P.op("dve", lambda e: e.tensor_reduce(out=lam.t[:, 0:1], in_=F[0].t[:, 0:64], axis=AX.X, op=ALU.add), r=[F[0]], w=[lam])
        tt("dve", F[0].t[:, 0:64], lv.t[:, 128:192], lv.t[:, 192:256], ALU.mult, r=[lv], w=[F[0]])
        P.op("dve", lambda e: e.tensor_reduce(out=lam.t[:, 1:2], in_=F[0].t[:, 0:64], axis=AX.X, op=ALU.add), r=[F[0]], w=[lam])
        act(lam.t[:, 0:2], lam.t[:, 0:2], AF.Exp, r=[lam], w=[lam])
        tt("dve", lam.t[:, 2:3], lam.t[:, 1:2], lam.t[:, 0:1], ALU.subtract, r=[lam], w=[lam])
        ts("dve", lam.t[:, 3:4], lam.t[:, 2:3], -LAM_INIT, ALU.add, r=[lam], w=[lam])
        gd08 = sb("gd08", [128, 1], F32)
        ts("dve", gd08.t[:], PR["gdiff"].t[:], 1.0 - LAM_INIT, ALU.mult, r=[PR["gdiff"]], w=[gd08])
        P.op("pool", lambda e: e.memset(mvb.t[:, :, :, 128:256], 1.0), w=[mvb])

        _dbg = {}
        def src_slab(i):
            if i < 6:
                return dram["w_in"].ap().rearrange("(k p) c -> p k c", p=128)[:, :, i * 512:(i + 1) * 512], "gpre"
            if i < 8:
                j = i - 6
                return dram["w_out"].ap().rearrange("(k p) c -> p k c", p=128)[:, :, j * 512:(j + 1) * 512], None
            j = i - 8
            g, q = j // 4, j % 4
            if q < 2:
                s_ = 2 * g + q
                return dram["w_up"].ap().rearrange("(k p) c -> p k c", p=128)[:, :, s_ * 512:(s_ + 1) * 512], "gmlp"
            cg = q - 2
            return dram["w_down"].ap().rearrange("(g k p) c -> p g k c", g=4, k=8, p=128)[:, g, :, cg * 512:(cg + 1) * 512], None

        stg = [y, xc, kT, vd]
        stg_ap = [y.t[:].rearrange("p a (b c) -> p (a b) c", b=2), xc.t[:].rearrange("p a (b c) -> p (a b) c", b=2),
                  kT.t.bitcast(F32)[:].rearrange("p a (b c) -> p (a b) c", b=2), vd.t.bitcast(F32)[:].rearrange("p (a b) c -> p a (b c)", b=2)]
        NSTG = 4
        cast_eng = ["dve", "pool"]

        def pro_load(i):
            src, gname = src_slab(i)
            sbuf_ = stg[i % NSTG]
            sap = stg_ap[i % NSTG]
            P.dma("sp", (lambda e, sap=sap, src=src: e.dma_start(out=sap, in_=src)), sbuf_[0], w=[sbuf_])

        for i in range(NSTG):
            pro_load(i)
        for i in range(24):
            src, gname = src_slab(i)
            sbuf_ = stg[i % NSTG]
            sap = stg_ap[i % NSTG]
            slot = wr[i % NW]
            if gname is None:
                for hh in range(2):
                    cp("pool" if hh == 0 else "dve", slot.t[:, hh * 4:(hh + 1) * 4, :], sap[:, hh * 4:(hh + 1) * 4, :], r=[sbuf_], w=[slot])
            else:
                for k in range(8):
                    if k % 2 == 0:
                        ts("dve", slot.t[:, k, :], sap[:, k, :], PR[gname].t[:, k:k + 1], ALU.mult, r=[sbuf_, PR[gname]], w=[slot])
                    else:
                        act(slot.t[:, k, :], sap[:, k, :], AF.Copy, r=[sbuf_, PR[gname]], w=[slot], scale=PR[gname].t[:, k:k + 1])
            P.dma("act", (lambda e, slot=slot, i=i: e.dma_start(out=dram["wbf"].ap()[i], in_=slot.t[:])), slot[0], r=[slot], w=[wbf_trk[i]])
            if i + NSTG < 24:
                pro_load(i + NSTG)
        gsrc = dram["w_in"].ap().rearrange("(k p) c -> p k c", p=128)[:, :, 3072:3080]
        P.dma("sp", lambda e: e.dma_start(out=wgs.t[:], in_=gsrc), wgs[0], w=[wgs])
        for k in range(8):
            ts("dve", wg.t[:, k, :], wgs.t[:, k, :], PR["gpre"].t[:, k:k + 1], ALU.mult, r=[wgs, PR["gpre"]], w=[wg])

        wstate = {"issued": 0, "cur": 0}
        ORDER = [2, 4, 0, 1, 3, 5, 6, 7] + [8, 9, 12, 13, 10, 11, 16, 17, 14, 15, 20, 21, 18, 19, 22, 23]
        total_slabs = 24 * nseq * NCH

        def w_issue():
            i = wstate["issued"]
            if i >= total_slabs:
                return
            slot = wr[i % NW]
            sl = ORDER[i % 24]
            P.dma("sp", (lambda e, slot=slot, sl=sl: e.dma_start(out=slot.t[:], in_=dram["wbf"].ap()[sl])), slot[0], r=[wbf_trk[sl]], w=[slot])
            wstate["issued"] += 1

        def w_get(expect):
            i = wstate["cur"]
            assert ORDER[i % 24] == expect, (i, expect)
            while wstate["issued"] <= i:
                w_issue()
            return wr[i % NW]

        def w_done():
            wstate["cur"] += 1
            while wstate["issued"] < min(wstate["cur"] + NW, total_slabs):
                w_issue()

        def rstd_from_ssq(stat_b, col, n, width):
            a = stat_b.t[:, col:col + n]
            act(a, a, AF.Ln, r=[stat_b], w=[stat_b], bias=C_eps.t[:, 0:1], scale=1.0 / width)
            act(a, a, AF.Exp, r=[stat_b], w=[stat_b], scale=-0.5)

        C_eps = sb("c_eps", [128, 2], F32)
        P.op("pool", lambda e: e.memset(C_eps.t[:, 0:1], EPS), w=[C_eps])
        P.op("pool", lambda e: e.memset(C_eps.t[:, 1:2], 1.0), w=[C_eps])
        eps_ap = C_eps.t[:, 0:1]
        one_ap = C_eps.t[:, 1:2]

        junk = sb("junk", [128, 1024], BF16)

        def norm_stats(t, stb, col):
            act(junk.t[:], xc.t[:, t, :], AF.Square, r=[xc[t]], w=[stb], accum_out=stb.t[:, col:col + 1])

        def norm_apply(t, stb, col):
            i = ring_state["hb"]
            ring_state["hb"] = (i + 1) % 3
            hbb = hb[i]
            ts("dve", hbb.t[:], xc.t[:, t, :], stb.t[:, col:col + 1], ALU.mult, r=[xc[t], stb], w=[hbb])
            return hbb

        def norm_transpose(t, hbb):
            pi = ringA()
            pt = PSbf[pi].t
            for k in range(8):
                P.op("pe", (lambda e, k=k: e.transpose(out=pt[:, k * 128:(k + 1) * 128], in_=hbb.t[:, k * 128:(k + 1) * 128], identity=C["ident_bf"].t[:])),
                     r=[hbb, C["ident_bf"]], w=[PS[pi]])
            cp("act", hT.t[:, :, t * 128:(t + 1) * 128], pt[:, 0:1024].rearrange("p (k c) -> p k c", k=8), r=[PS[pi]], w=[hT[t]])

        def norm_all_to_hT():
            stb = stat[0]
            for t in range(4):
                norm_stats(t, stb, t)
            rstd_from_ssq(stb, 0, 4, D)
            for t in range(4):
                hbb = norm_apply(t, stb, t)
                norm_transpose(t, hbb)

        def ynorm_residual(t, gname):
            stb = stat[2 + (t % 2)]
            ys = y.t[:, t, :]
            act(junk.t[:], ys, AF.Square, r=[y[t]], w=[stb], accum_out=stb.t[:, 0:1])
            rstd_from_ssq(stb, 0, 1, D)
            stt_(ys, ys, stb.t[:, 0:1], PR[gname].t[:], ALU.mult, ALU.mult, r=[y[t], stb, PR[gname]], w=[y[t]])
            tt("pool" if t % 2 else "dve", xc.t[:, t, :], xc.t[:, t, :], ys, ALU.add, r=[xc[t], y[t]], w=[xc[t]])

        def chunk(b, c):
            tok0 = b * S + c * CH
            first = (c == 0)
            for t in range(4):
                P.dma("sp", (lambda e, t=t: e.dma_start(out=xc.t[:, t, :], in_=dram["x"].ap()[tok0 + t * 128: tok0 + (t + 1) * 128, :])), xc[t], w=[xc[t]])
            P.dma("sp", lambda e: e.dma_start(out=cs.t[:, 0, :], in_=dram["cos_t"].ap()[:, c * 512:(c + 1) * 512]), cs[0], w=[cs])
            P.dma("sp", lambda e: e.dma_start(out=cs.t[:, 1, :], in_=dram["sin_t"].ap()[:, c * 512:(c + 1) * 512]), cs[0], w=[cs])
            _stage(1)
            norm_all_to_hT()
            _stage(2)

            def modeB(slab, j):
                pi = ringA()
                for k in range(8):
                    mm(PS[pi].t[:, :], slab.t[:, k, j * 128:(j + 1) * 128], hT.t[:, k, :], k == 0, k == 7, r=[slab, hT], w=[PS[pi]])
                return pi

            def rope(pi, dst_ap, dst_trk, par):
                xbb = ET[par]
                cp("act", xbb.t[:], PS[pi].t[:, :], r=[PS[pi]], w=[xbb])
                _stage(2111)
                p2 = ringA()
                mm(PS[p2].t[:, :], C["perm_bf"].t[:], xbb.t[:], True, True, r=[C["perm_bf"], xbb], w=[PS[p2]])
                _stage(2112)
                t1, t2 = F[2 * par], F[2 * par + 1]
                tt("dve", t1.t[:, 0:512], PS[pi].t[:, :], cs.t[:, 0, :], ALU.mult, r=[PS[pi], cs], w=[t1])
                _stage(2113)
                tt("dve", t2.t[:, 0:512], PS[p2].t[:, :], cs.t[:, 1, :], ALU.mult, r=[PS[p2], cs], w=[t2])
                _stage(2114)
                tt(ROPE_ADD_ENG, dst_ap, t1.t[:, 0:512], t2.t[:, 0:512], ALU.add, r=[t1, t2], w=dst_trk)

            _stage(22)
            slab = w_get(2)
            for t in range(4):
                pi = ringA()
                for k in range(8):
                    mm(PS[pi].t[:, :], hT.t[:, k, t * 128:(t + 1) * 128], slab.t[:, k, :], k == 0, k == 7, r=[hT[t], slab], w=[PS[pi]])
                cp("dve", vd.t[:, c * 4 + t, :], PS[pi].t[:, :], r=[PS[pi]], w=[vd[c * 4 + t]])
            w_done()
            _stage(24)
            slab = w_get(4)
            for t in range(4):
                pi = ringA()
                for k in range(8):
                    mm(PS[pi].t[:, :], hT.t[:, k, t * 128:(t + 1) * 128], slab.t[:, k, :], k == 0, k == 7, r=[hT[t], slab], w=[PS[pi]])
                cp("dve", mvb.t[:, t, :, 0:128], PS[pi].t[:, :].rearrange("p (h v) -> p h v", h=4), r=[PS[pi]], w=[mvb[t]])
                pg = ringA()
                for k in range(8):
                    mm(PS[pg].t[:, 0:8], hT.t[:, k, t * 128:(t + 1) * 128], wg.t[:, k, :], k == 0, k == 7, r=[hT[t], wg], w=[PS[pg]])
                tt("dve", gate.t[:, t, :], PS[pg].t[:, 0:8], PR["gbias"].t[:], ALU.add, r=[PS[pg], PR["gbias"]], w=[gate[t]])
                act(lf.t[:, t, :], gate.t[:, t, 4:8], AF.Exp, r=[gate[t]], w=[lf[t]], scale=-1.0)
                act(lf.t[:, t, :], lf.t[:, t, :], AF.Ln, r=[lf[t]], w=[lf[t]], bias=one_ap, scale=1.0)
                ts("dve", lf.t[:, t, :], lf.t[:, t, :], -1.0, ALU.mult, r=[lf[t]], w=[lf[t]])
            w_done()
            slab = w_get(0)
            _stage(210)
            for h in range(4):
                pi = modeB(slab, h)
                _stage(211)
                rope(pi, qT.t[:, h, :], [qT[h]], h % 2)
                _stage(212)
            w_done()
            _stage(21)
            slab = w_get(1)
            for h in range(4):
                pi = modeB(slab, h)
                rope(pi, kT.t[:, h, c * 512:(c + 1) * 512], [kT[h * 4 + c]], h % 2)
            w_done()
            _stage(23)
            slab = w_get(3)
            for j in range(4):
                pi = modeB(slab, j)
                raw, acc, sg = F[4], F[5], F[6]
                if first:
                    P.op("pool", lambda e: e.memset(raw.t[:, 0:3], 0.0), w=[raw])
                else:
                    cp("pool", raw.t[:, 0:3], hal.t[:, j, 0:3], r=[hal[j]], w=[raw])
                cp("act", raw.t[:, 3:515], PS[pi].t[:, :], r=[PS[pi]], w=[raw])
                cp("pool", hal.t[:, j, 0:3], raw.t[:, 512:515], r=[raw], w=[hal[j]])
                cw = PR["convw"]
                ts("dve", acc.t[:, 0:512], raw.t[:, 3:515], cw.t[:, j * 4 + 3:j * 4 + 4], ALU.mult, r=[raw, cw, PR["convb"]], w=[acc],
                   s2=PR["convb"].t[:, j:j + 1], op1=ALU.add)
                for tap in (2, 1, 0):
                    stt_(acc.t[:, 0:512], raw.t[:, tap:tap + 512], cw.t[:, j * 4 + tap:j * 4 + tap + 1], acc.t[:, 0:512], ALU.mult, ALU.add,
                         r=[raw, cw, acc], w=[acc])
                act(sg.t[:, 0:512], acc.t[:, 0:512], AF.Exp, r=[acc], w=[sg], scale=-1.0)
                act(sg.t[:, 0:512], sg.t[:, 0:512], AF.Ln, r=[sg], w=[sg], bias=one_ap, scale=1.0)
                act(sg.t[:, 0:512], sg.t[:, 0:512], AF.Exp, r=[sg], w=[sg], scale=-1.0)
                if j < 2:
                    stt_(mqT.t[:, j, :], acc.t[:, 0:512], 0.125, sg.t[:, 0:512], ALU.mult, ALU.mult, r=[acc, sg], w=[mqT[j]])
                else:
                    tt("dve", mkT.t[:, j - 2, :], acc.t[:, 0:512], sg.t[:, 0:512], ALU.mult, r=[acc, sg], w=[mkT[j - 2]])
            w_done()
            slab_mo = w_get(5)
            _stage(3)

            for h in range(4):
                mlstm_prep(b, c, h)
                fin = attention(b, c, h, mid=(lambda h=h: mlstm_prep2(b, c, h)))
                _stage(4)
                mlstm_tile(b, c, h)
                fin()
                _stage(5)
            mlstm_post_all(slab_mo)
            w_done()

            _stage(6)
            s0 = w_get(6)
            assert wstate["issued"] > wstate["cur"] + 1
            s1 = wr[(wstate["cur"] + 1) % NW]
            pend = []
            for t in range(4):
                for cg, sl_ in ((0, s0), (1, s1)):
                    pi = ringA()
                    for k in range(8):
                        mm(PS[pi].t[:, :], oT.t[:, k, t * 128:(t + 1) * 128], sl_.t[:, k, :], k == 0, k == 7, r=[oT, sl_], w=[PS[pi]])
                    cp("act" if cg == 0 else "dve", y.t[:, t, cg * 512:(cg + 1) * 512], PS[pi].t[:, :], r=[PS[pi]], w=[y[t]])
                if len(pend) == 2:
                    norm_transpose(*pend.pop(0))
                ynorm_residual(t, "gpost_b")
                stb = stat[t % 2]
                norm_stats(t, stb, 4)
                rstd_from_ssq(stb, 4, 1, D)
                hbb = norm_apply(t, stb, 4)
                pend.append((t, hbb))
            w_done()
            assert w_get(7) is s1
            w_done()
            for pp in pend:
                norm_transpose(*pp)
            _stage(7)
            def mlp_up(g):
                uT = uTb[g % 2]
                for s_ in range(2):
                    slab = w_get(8 + g * 4 + s_)
                    for j in range(4):
                        pi = modeB(slab, j)
                        rr = F[(s_ * 4 + j) % 4]
                        act(rr.t[:, 0:512], PS[pi].t[:, :], AF.Relu, r=[PS[pi]], w=[rr])
                        tt("pool" if j % 2 else "dve", uT.t[:, s_ * 4 + j, :], rr.t[:, 0:512], rr.t[:, 0:512], ALU.mult, r=[rr], w=[uT[s_ * 4 + j]])
                    w_done()

            def mlp_down(g):
                uT = uTb[g % 2]
                for cg in range(2):
                    slab = w_get(8 + g * 4 + 2 + cg)
                    for t in range(4):
                        pi = ringA()
                        for k in range(8):
                            mm(PS[pi].t[:, :], uT.t[:, k, t * 128:(t + 1) * 128], slab.t[:, k, :], k == 0, k == 7, r=[uT, slab], w=[PS[pi]])
                        ydst = y.t[:, t, cg * 512:(cg + 1) * 512]
                        if g == 0:
                            cp("act", ydst, PS[pi].t[:, :], r=[PS[pi]], w=[y[t]])
                        else:
                            tt("dve", ydst, ydst, PS[pi].t[:, :], ALU.add, r=[y[t], PS[pi]], w=[y[t]])
                    w_done()

            mlp_up(0)
            mlp_up(1)
            mlp_down(0)
            mlp_up(2)
            mlp_down(1)
            mlp_up(3)
            mlp_down(2)
            mlp_down(3)
            for t in range(4):
                ynorm_residual(t, "gmlppost_b")
                P.dma("sp", (lambda e, t=t: e.dma_start(out=dram["out"].ap()[tok0 + t * 128: tok0 + (t + 1) * 128, :], in_=xc.t[:, t, :])),
                      xc[t], r=[xc[t]], store=True)

        def attention(b, c, h, mid=None):
            nkb = 4 * c + 4
            ON = [PS[4], PS[5]]
            DEN = [PS[6], PS[7]]
            ktr = [kT[h * 4 + cc] for cc in range(c + 1)]
            et_state = {"i": 0}

            def qk(kb):
                qlo = max(0, kb - 4 * c) * 128
                n = 512 - qlo
                res = []
                for m in range(2):
                    pi = ringA()
                    mm(PS[pi].t[:, 0:n], kT.t[m * 64:(m + 1) * 64, h, kb * 128:(kb + 1) * 128], qT.t[m * 64:(m + 1) * 64, h, qlo:512], True, True,
                       r=[kT[h * 4 + kb // 4], qT[h]], w=[PS[pi]])
                    ei = et_state["i"]
                    et_state["i"] = (ei + 1) % 4
                    et = ET[ei]
                    act(et.t[:, 0:n], PS[pi].t[:, 0:n], AF.Exp, r=[PS[pi]], w=[et], scale=0.125)
                    if kb >= 4 * c:
                        tt("pool", et.t[:, 0:128], et.t[:, 0:128], C["mask_bf"].t[:], ALU.mult, r=[et, C["mask_bf"]], w=[et])
                    res.append((et, qlo, n))
                return res

            def pv(kb, res):
                for m in range(2):
                    et, qlo, n = res[m]
                    mm(ON[m].t[:, qlo:512], vd.t[:, kb, h * 128:(h + 1) * 128], et.t[:, 0:n], kb == 0, kb == nkb - 1, r=[vd[kb], et], w=[ON[m]])
                    mm(DEN[m].t[:, qlo:512], C["ones_bf"].t[:], et.t[:, 0:n], kb == 0, kb == nkb - 1, r=[C["ones_bf"], et], w=[DEN[m]])

            prev = qk(0)
            for kb in range(nkb):
                nxt = qk(kb + 1) if kb + 1 < nkb else None
                pv(kb, prev)
                prev = nxt
                if kb == 0 and mid is not None:
                    mid()
            T1, T2 = F[0], F[1]
            a1, a2 = T1.t[:, 0:512], T2.t[:, 0:512]
            act(a1, DEN[0].t[:, :], AF.Ln, r=[DEN[0]], w=[T1])
            act(a1, a1, AF.Exp, r=[T1], w=[T1], scale=-1.0)
            act(a2, DEN[1].t[:, :], AF.Ln, r=[DEN[1]], w=[T2])
            act(a2, a2, AF.Exp, r=[T2], w=[T2], scale=-1.0)
            tt("dve", a1, ON[0].t[:, :], a1, ALU.mult, r=[ON[0], T1], w=[T1])
            tt("dve", a2, ON[1].t[:, :], a2, ALU.mult, r=[ON[1], T2], w=[T2])
            stt_(a1, a2, lam.t[:, 3:4], a1, ALU.mult, ALU.add, r=[T1, T2, lam], w=[T1])
            tt("dve", a2, a1, a1, ALU.mult, r=[T1], w=[T2])

            def finish():
                pi = ringA()
                mm(PS[pi].t[:, :], C["ones_f"].t[:], a2, True, True, r=[C["ones_f"], T2], w=[PS[pi]])
                act(a2, PS[pi].t[:, :], AF.Ln, r=[PS[pi]], w=[T2], bias=eps_ap, scale=1.0 / 128)
                act(a2, a2, AF.Exp, r=[T2], w=[T2], scale=-0.5)
                stt_(oT.t[:, h, :], a1, gd08.t[:, 0:1], a2, ALU.mult, ALU.mult, r=[T1, T2, gd08], w=[oT[h]])
            return finish

        def mlstm_prep(b, c, t):
            first = (c == 0 and t == 0)
            tc_ = slice(t * 128, (t + 1) * 128)
            if first:
                P.op("pool", lambda e: e.memset(stt.t[:], 0.0), w=[stt])
                P.op("pool", lambda e: e.memset(Cbf.t[:], 0.0), w=[Cbf])
            lft = lf.t[:, t, :]
            pg = ringA()
            mm(PS[pg].t[:, 0:4], C["triu_f"].t[:], lft, True, True, r=[C["triu_f"], lf[t]], w=[PS[pg]])
            mm(PS[pg].t[:, 4:8], C["ones_f"].t[:], lft, True, True, r=[C["ones_f"], lf[t]], w=[PS[pg]])
            bc = sm.t[:, 0:4]
            wk = sm.t[:, 4:8]
            eG = sm.t[:, 8:12]
            tt("dve", bc, gate.t[:, t, 0:4], PS[pg].t[:, 0:4], ALU.subtract, r=[gate[t], PS[pg]], w=[sm])
            tt("dve", wk, PS[pg].t[:, 4:8], bc, ALU.add, r=[PS[pg], sm], w=[sm])
            act(wk, wk, AF.Exp, r=[sm], w=[sm])
            act(eG, PS[pg].t[:, 4:8], AF.Exp, r=[PS[pg]], w=[sm])
            RL, EB, DT, DM = F[2], F[3], F[4], F[5]
            for h in range(4):
                ts("dve", RL.t[:, h * 128:(h + 1) * 128], C["triu_f"].t[:], lf.t[:, t, h:h + 1], ALU.mult, r=[C["triu_f"], lf[t]], w=[RL])

        def mlstm_prep2(b, c, t):
            tc_ = slice(t * 128, (t + 1) * 128)
            RL, EB, DT, DM = F[2], F[3], F[4], F[5]
            pB = ringA()
            mm(PS[pB].t[:, :], C["ones_f"].t[:], RL.t[:, 0:512], True, True, r=[C["ones_f"], RL], w=[PS[pB]])
            act(EB.t[:, 0:512], PS[pB].t[:, :], AF.Exp, r=[PS[pB]], w=[EB])
            pBm = ringA()
            mm(PS[pBm].t[:, :], C["ones_f"].t[:], RL.t[:, 0:512], True, False, r=[C["ones_f"], RL], w=[PS[pBm]])
            mm(PS[pBm].t[:, :], C["ident_f"].t[:], C["maskneg4"].t[:], False, True, r=[C["ident_f"], C["maskneg4"]], w=[PS[pBm]])
            for h in range(4):
                hs = slice(h * 128, (h + 1) * 128)
                act(DT.t[:, hs], PS[pBm].t[:, hs], AF.Exp, r=[PS[pBm], sm], w=[DT], bias=sm.t[:, h:h + 1], scale=1.0)
            pT = ringA()
            for j in range(2):
                P.op("pe", (lambda e, j=j: e.transpose(out=PSbf[pT].t[:, j * 128:(j + 1) * 128], in_=mkT.t[:, j, tc_], identity=C["ident_bf"].t[:])),
                     r=[mkT[j], C["ident_bf"]], w=[PS[pT]])
            for h in range(4):
                ts("dve", ktil.t[:, h * 64:(h + 1) * 64], PSbf[pT].t[:, h * 64:(h + 1) * 64], sm.t[:, 4 + h:5 + h], ALU.mult, r=[PS[pT], sm], w=[ktil])
            for h in range(4):
                base = (h % 2) * 64
                j = h // 2
                ps_ = slice(base, base + 64)
                hs = slice(h * 128, (h + 1) * 128)
                tt("dve", qsb.t[ps_, h, :], mqT.t[ps_, j, tc_], EB.t[ps_, hs], ALU.mult, r=[mqT[j], EB], w=[qsb[h]])
            pSb = [ringA(), ringA()]
            for h in range(4):
                base = (h % 2) * 64
                j = h // 2
                ps_ = slice(base, base + 64)
                hs = slice(h * 128, (h + 1) * 128)
                pb_ = pSb[h % 2]
                mm(PS[pb_].t[:, hs], mkT.t[ps_, j, tc_], mqT.t[ps_, j, tc_], True, True, r=[mkT[j], mqT[j]], w=[PS[pb_]])
            for h in range(4):
                hs = slice(h * 128, (h + 1) * 128)
                pb_ = pSb[h % 2]
                tt("dve", AT.t[:, h, :], PS[pb_].t[:, hs], DT.t[:, hs], ALU.mult, r=[PS[pb_], DT], w=[AT[h]])

        def mlstm_tile(b, c, t):
            tc_ = slice(t * 128, (t + 1) * 128)
            RL, EB, DT, DM = F[2], F[3], F[4], F[5]
            pN, pD, pS = 4, 5, 6
            HB = [((h % 2) * 64, h // 2, slice((h % 2) * 64, (h % 2) * 64 + 64), slice(h * 128, (h + 1) * 128)) for h in range(4)]
            pUs = []
            for h in range(4):
                base, j, ps_, hs = HB[h]
                mm(PS[pN].t[:, hs], mvb.t[:, t, h, 0:128], AT.t[:, h, :], True, False, r=[mvb[t], AT[h]], w=[PS[pN]])
                mm(PS[pN].t[:, hs], Cbf.t[ps_, h, 0:128], qsb.t[ps_, h, :], False, True, r=[Cbf[h], qsb[h]], w=[PS[pN]])
                mm(PS[pD].t[:, hs], C["ones_bf"].t[:], AT.t[:, h, :], True, False, r=[C["ones_bf"], AT[h]], w=[PS[pD]])
                mm(PS[pD].t[:, hs], Cbf.t[ps_, h, 128:256], qsb.t[ps_, h, :], False, True, r=[Cbf[h], qsb[h]], w=[PS[pD]])
                pU = ringA()
                pUs.append(pU)
                mm(PS[pU].t[:, 0:256], ktil.t[:, j * 128:(j + 1) * 128], mvb.t[:, t, h, :], True, True, r=[ktil, mvb[t]], w=[PS[pU]])
            for h in range(4):
                base, j, ps_, hs = HB[h]
                pU = pUs[h]
                stt_(stt.t[ps_, h, :], stt.t[ps_, h, :], sm.t[ps_, 8 + h:9 + h], PS[pU].t[ps_, 0:256], ALU.mult, ALU.add,
                     r=[stt[h], sm, PS[pU]], w=[stt[h]])
                cp("pool", Cbf.t[ps_, h, :], stt.t[ps_, h, :], r=[stt[h]], w=[Cbf[h]])
            act(DM.t[:, 0:512], PS[pD].t[:, :], AF.Abs, r=[PS[pD]], w=[DM])
            ts("dve", DM.t[:, 0:512], DM.t[:, 0:512], 1.0, ALU.max, r=[DM], w=[DM])
            act(DM.t[:, 0:512], DM.t[:, 0:512], AF.Ln, r=[DM], w=[DM])
            act(DM.t[:, 0:512], DM.t[:, 0:512], AF.Exp, r=[DM], w=[DM], scale=-1.0)
            tt("dve", hmS.t[:, :, tc_], PS[pN].t[:, :].rearrange("p (h v) -> p h v", h=4), DM.t[:, 0:512].rearrange("p (h v) -> p h v", h=4), ALU.mult,
               r=[PS[pN], DM], w=[hmS])

        def mlstm_post_all(slab_mo):
            SQ = [F[0], F[1], F[6], F[7]]
            SG = [F[2], F[3], F[4], F[5]]
            for h in range(4):
                hm = hmS.t[:, h, :]
                tt("pool" if h % 2 else "dve", SQ[h].t[:, 0:512], hm, hm, ALU.mult, r=[hmS[h]], w=[SQ[h]])
            for h in range(4):
                sq = SQ[h].t[:, 0:512]
                pi = ringA()
                mm(PS[pi].t[:, :], C["ones_f"].t[:], sq, True, True, r=[C["ones_f"], SQ[h]], w=[PS[pi]])
                act(sq, PS[pi].t[:, :], AF.Ln, r=[PS[pi]], w=[SQ[h]], bias=eps_ap, scale=1.0 / 128)
                act(sq, sq, AF.Exp, r=[SQ[h]], w=[SQ[h]], scale=-0.5)
                stt_(sq, hmS.t[:, h, :], PR["gml"].t[:, h:h + 1], sq, ALU.mult, ALU.mult, r=[hmS[h], PR["gml"], SQ[h]], w=[SQ[h]])
            for h in range(4):
                pm = ringA()
                for k in range(8):
                    mm(PS[pm].t[:, :], slab_mo.t[:, k, h * 128:(h + 1) * 128], hT.t[:, k, :], k == 0, k == 7, r=[slab_mo, hT], w=[PS[pm]])
                sg = SG[h].t[:, 0:512]
                act(sg, PS[pm].t[:, :], AF.Exp, r=[PS[pm]], w=[SG[h]], scale=-1.0)
                act(sg, sg, AF.Ln, r=[SG[h]], w=[SG[h]], bias=one_ap, scale=1.0)
                act(sg, sg, AF.Exp, r=[SG[h]], w=[SG[h]], scale=-1.0)
                tt("pool" if h % 2 else "dve", oT.t[:, 4 + h, :], SQ[h].t[:, 0:512], sg, ALU.mult, r=[SQ[h], SG[h]], w=[oT[4 + h]])

        try:
            for b in range(nseq):
                for c in range(NCH):
                    chunk(b, c)
        except _Stop:
            pass

        P.emit(st)
    return nc


_CACHE = {}


def _run(inputs, n_cores, nseq):
    consts = _consts()
    params = _params(inputs)
    x = np.ascontiguousarray(np.asarray(inputs["x"], dtype=np.float32))
    B = x.shape[0]
    assert B == n_cores * nseq
    key = (nseq,)
    if key not in _CACHE:
        _CACHE[key] = build(nseq)
    nc = _CACHE[key]
    shared = {
        "w_in": np.ascontiguousarray(np.asarray(inputs["w_in"][0], dtype=np.float32)),
        "w_out": np.ascontiguousarray(np.asarray(inputs["w_out"][0], dtype=np.float32)),
        "w_up": np.ascontiguousarray(np.asarray(inputs["w_up"][0], dtype=np.float32)),
        "w_down": np.ascontiguousarray(np.asarray(inputs["w_down"][0], dtype=np.float32)),
    }
    shared.update(consts)
    shared.update(params)
    in_maps = []
    for i in range(n_cores):
        m = dict(shared)
        m["x"] = x[i * nseq:(i + 1) * nseq].reshape(nseq * S, D)
        in_maps.append(m)
    res = run_bass_kernel_spmd(nc, in_maps, core_ids=list(range(n_cores)))
    outs = [np.asarray(r["out"]).reshape(nseq, S, D) for r in res.results]
    return np.concatenate(outs, axis=0).astype(np.float32)


def kernel(**inputs):
    return _run(inputs, 8, 4)
```

```python
import math
import numpy as np
import ml_dtypes
import concourse.bass as bass
import concourse.mybir as mybir
from concourse.bass_utils import run_bass_kernel_spmd

F32 = mybir.dt.float32
BF16 = mybir.dt.bfloat16
AF = mybir.ActivationFunctionType
ALU = mybir.AluOpType
AX = mybir.AxisListType

S = 2048
D = 1024
CH = 512
NCH = S // CH
DFF = 4096
EPS = 1e-6
LAM_INIT = 0.8 - 0.6 * math.exp(-0.3 * 0)
NW = 4
DBG_STAGE = None
ROPE_ADD_ENG = "pool"


class _Stop(Exception):
    pass


def _stage(n):
    if DBG_STAGE is not None and DBG_STAGE == n:
        raise _Stop()


class Trk:
    __slots__ = ("w", "r", "dcnt", "name", "sem", "excl")

    def __init__(self, name):
        self.excl = False
        self.w = None
        self.r = {}
        self.dcnt = 0
        self.name = name
        self.sem = None


class Buf:
    def __init__(self, t, name, nsub=1):
        self.t = t
        self.name = name
        self.trk = [Trk(f"{name}.{i}") for i in range(nsub)]

    def __getitem__(self, i):
        return self.trk[i]


def _trks(lst):
    out = []
    for x in lst:
        if isinstance(x, Buf):
            out.extend(x.trk)
        elif isinstance(x, Trk):
            out.append(x)
        else:
            out.extend(_trks(x))
    return out


class Prog:
    ENG = ["pe", "act", "dve", "pool", "sp"]

    def __init__(self, nc):
        self.nc = nc
        self.ops = {e: [] for e in self.ENG}
        self.owners = []
        self.stores = []

    def _deps(self, eng, r, w):
        deps = {}

        def add(tok):
            key, val = tok
            if key[0] == "E" and key[1] == "pe" and eng == "pe":
                return
            if deps.get(key, -1) < val:
                deps[key] = val

        for t in r:
            if t.w is not None:
                add(t.w)
            if t.excl:
                for k, v in t.r.items():
                    if k != ("E", eng):
                        add((k, v))
        for t in w:
            if t.w is not None:
                add(t.w)
            for k, v in t.r.items():
                add((k, v))
        return deps

    def _commit(self, tok, r, w):
        key, val = tok
        for t in r:
            if t.r.get(key, -1) < val:
                t.r[key] = val
        for t in w:
            t.w = tok
            t.r = {}

    def op(self, eng, fn, r=(), w=()):
        r = _trks(r)
        w = _trks(w)
        deps = self._deps(eng, r, w)
        idx = len(self.ops[eng])
        tok = (("E", eng), idx)
        self.ops[eng].append(dict(fn=fn, deps=deps, kind="E", idx=idx))
        self._commit(tok, r, w)

    def dma(self, eng, fn, owner, r=(), w=(), store=False):
        r = _trks(r)
        w = _trks(w)
        deps = self._deps(eng, r, w)
        if owner.sem is None:
            owner.sem = "pending"
            self.owners.append(owner)
        key = ("D", id(owner))
        if owner.dcnt > 0:
            if deps.get(key, -1) < owner.dcnt:
                deps[key] = owner.dcnt
        owner.dcnt += 1
        tok = (key, owner.dcnt)
        self.ops[eng].append(dict(fn=fn, deps=deps, kind="D", owner=owner))
        self._commit(tok, r, w)
        if store:
            self.stores.append(tok)

    def emit(self, stack):
        nc = self.nc
        sig = {e: set() for e in self.ENG}
        for e in self.ENG:
            for o in self.ops[e]:
                for key, val in o["deps"].items():
                    if key[0] == "E":
                        sig[key[1]].add(val)
        for tok in self.stores:
            pass
        rank = {}
        for e in self.ENG:
            rank[e] = {idx: i + 1 for i, idx in enumerate(sorted(sig[e]))}
        esem = {e: stack.enter_context(nc.semaphore(f"sem_{e}")) for e in self.ENG}
        osem = {}
        for i, o in enumerate(self.owners):
            osem[id(o)] = stack.enter_context(nc.semaphore(f"dsem_{i}"))
        block = stack.enter_context(nc.Block())
        hooks = dict(pe=block.tensor, act=block.scalar, dve=block.vector, pool=block.gpsimd, sp=block.sync)
        stores = self.stores

        def make(e):
            def body(engine):
                waited = {}
                for o in self.ops[e]:
                    for key, val in o["deps"].items():
                        if key[0] == "E":
                            sem = esem[key[1]]
                            v = rank[key[1]][val]
                        else:
                            sem = osem[key[1]]
                            v = 16 * val
                        if waited.get(key, 0) < v:
                            engine.wait_ge(sem, v)
                            waited[key] = v
                    inst = o["fn"](engine)
                    if o["kind"] == "E":
                        if o["idx"] in rank[e]:
                            inst.then_inc(esem[e], 1)
                    else:
                        inst.then_inc(osem[id(o["owner"])], 16)
                if e == "sp":
                    fin = {}
                    for key, val in stores:
                        fin[key] = max(fin.get(key, 0), val)
                    for key, val in fin.items():
                        if waited.get(key, 0) < 16 * val:
                            engine.wait_ge(osem[key[1]], 16 * val)
            return body

        for e in self.ENG:
            hooks[e](make(e))


def _consts():
    bf = ml_dtypes.bfloat16
    c = {}
    eye = np.eye(128, dtype=np.float32)
    c["ident_bf"] = eye.astype(bf)
    c["ident_f"] = eye.copy()
    perm = np.zeros((128, 128), np.float32)
    for k in range(128):
        m = (k // 64) * 64 + ((k % 64) + 32) % 64
        perm[k, m] = 1.0
    c["perm_bf"] = perm.astype(bf)
    c["ones_bf"] = np.ones((128, 128), bf)
    c["ones_f"] = np.ones((128, 128), np.float32)
    kk = np.arange(128)[:, None]
    jj = np.arange(128)[None, :]
    triu = (kk <= jj).astype(np.float32)
    c["triu_f"] = triu
    c["mask_bf"] = triu.astype(bf)
    c["maskneg4"] = np.tile(np.where(kk > jj, -30000.0, 0.0).astype(np.float32), (1, 4))
    p = np.arange(128)
    inv = 10000.0 ** (-np.arange(0, 64, 2, dtype=np.float64) / 64.0)
    ang = np.arange(S, dtype=np.float64)[None, :] * inv[p % 32][:, None]
    sign = np.where((p % 64) < 32, -1.0, 1.0)[:, None]
    c["cos_t"] = np.cos(ang).astype(np.float32)
    c["sin_t"] = (sign * np.sin(ang)).astype(np.float32)
    return c


CONST_SPECS = [
    ("ident_bf", [128, 128], BF16), ("ident_f", [128, 128], F32), ("perm_bf", [128, 128], BF16),
    ("ones_bf", [128, 128], BF16), ("ones_f", [128, 128], F32), ("triu_f", [128, 128], F32),
    ("mask_bf", [128, 128], BF16), ("maskneg4", [128, 512], F32),
]
PARAM_SPECS = [
    ("gpre", [128, 8]), ("gmlp", [128, 8]), ("gpost_b", [128, 1024]), ("gmlppost_b", [128, 1024]),
    ("convw", [128, 16]), ("convb", [128, 4]), ("gbias", [128, 8]), ("lamv", [128, 256]),
    ("gdiff", [128, 1]), ("gml", [128, 4]),
]


def _params(inp):
    f = lambda a: np.ascontiguousarray(np.asarray(a, dtype=np.float32))
    p = {}
    p["gpre"] = f(inp["norm_mix_pre"][0].reshape(8, 128).T)
    p["gmlp"] = f(inp["norm_mlp_pre"][0].reshape(8, 128).T)
    p["gpost_b"] = f(np.broadcast_to(inp["norm_mix_post"][0][None, :], (128, 1024)))
    p["gmlppost_b"] = f(np.broadcast_to(inp["norm_mlp_post"][0][None, :], (128, 1024)))
    cw = np.asarray(inp["conv_w"][0])
    p["convw"] = f(cw.T.reshape(4, 128, 4).transpose(1, 0, 2).reshape(128, 16))
    p["convb"] = f(np.asarray(inp["conv_b"][0]).reshape(4, 128).T)
    gb = np.concatenate([np.asarray(inp["b_igate"][0]), np.asarray(inp["b_fgate"][0])])
    p["gbias"] = f(np.broadcast_to(gb[None, :], (128, 8)))
    lv = np.concatenate([np.asarray(inp[k][0]) for k in ("lambda_q1", "lambda_k1", "lambda_q2", "lambda_k2")])
    p["lamv"] = f(np.broadcast_to(lv[None, :], (128, 256)))
    p["gdiff"] = f(np.asarray(inp["diff_norm"][0])[:, None])
    p["gml"] = f(np.asarray(inp["mlstm_norm"][0]).T)
    return p


def build(nseq):
    from contextlib import ExitStack

    nc = bass.Bass("TRN2", target_bir_lowering=False)
    NT = nseq * S
    dram = {}
    dram["x"] = nc.dram_tensor("x", [NT, D], F32, kind="ExternalInput")
    dram["out"] = nc.dram_tensor("out", [NT, D], F32, kind="ExternalOutput")
    dram["w_in"] = nc.dram_tensor("w_in", [D, 3080], F32, kind="ExternalInput")
    dram["w_out"] = nc.dram_tensor("w_out", [D, D], F32, kind="ExternalInput")
    dram["w_up"] = nc.dram_tensor("w_up", [D, DFF], F32, kind="ExternalInput")
    dram["w_down"] = nc.dram_tensor("w_down", [DFF, D], F32, kind="ExternalInput")
    for name, shp, dt in CONST_SPECS:
        dram[name] = nc.dram_tensor(name, shp, dt, kind="ExternalInput")
    dram["cos_t"] = nc.dram_tensor("cos_t", [128, S], F32, kind="ExternalInput")
    dram["sin_t"] = nc.dram_tensor("sin_t", [128, S], F32, kind="ExternalInput")
    for name, shp in PARAM_SPECS:
        dram[name] = nc.dram_tensor(name, shp, F32, kind="ExternalInput")
    dram["wbf"] = nc.dram_tensor("wbf", [24, 128, 8, 512], BF16, kind="Internal")
    wbf_trk = [Trk(f"wbf{i}") for i in range(24)]

    with ExitStack() as st:
        P = Prog(nc)

        def sb(name, shape, dt, nsub=1):
            return Buf(st.enter_context(nc.sbuf_tensor("sb_" + name, shape, dt)), name, nsub)

        def psb(name, shape, dt):
            return Buf(st.enter_context(nc.psum_tensor(name, shape, dt)), name, 1)

        C = {name: sb(name, shp, dt) for name, shp, dt in CONST_SPECS}
        PR = {name: sb(name, shp, F32) for name, shp in PARAM_SPECS}
        const_owner = Trk("const_owner")
        for name in list(C) + list(PR):
            b = C.get(name) or PR[name]
            P.dma("sp", (lambda e, b=b, name=name: e.dma_start(out=b.t[:], in_=dram[name].ap())), const_owner, w=[b])

        kT = sb("kT", [128, 4, S], BF16, nsub=16)
        vd = sb("vd", [128, 16, 512], BF16, nsub=16)
        cs = sb("cs", [128, 2, 512], F32)
        xc = sb("xc", [128, 4, 1024], F32, nsub=4)
        hT = sb("hT", [128, 8, 512], BF16, nsub=4)
        qT = sb("qT", [128, 4, 512], BF16, nsub=4)
        mqT = sb("mqT", [128, 2, 512], BF16, nsub=2)
        mkT = sb("mkT", [128, 2, 512], BF16, nsub=2)
        hal = sb("hal", [128, 4, 4], F32, nsub=4)
        mvb = sb("mvb", [128, 4, 4, 256], BF16, nsub=4)
        ktil = sb("ktil", [128, 256], BF16)
        oT = sb("oT", [128, 8, 512], BF16, nsub=8)
        uTb = [sb("uT0", [128, 8, 512], BF16, nsub=8), sb("uT1", [128, 8, 512], BF16, nsub=8)]
        y = sb("y", [128, 4, 1024], F32, nsub=4)
        wr = [sb(f"wr{i}", [128, 8, 512], BF16) for i in range(NW)]
        wg = sb("wg", [128, 8, 8], BF16)
        wgs = sb("wgs", [128, 8, 8], F32)
        ET = [sb(f"et{i}", [128, 512], BF16) for i in range(4)]
        F = [sb(f"f{i}", [128, 520], F32) for i in range(8)]
        hmS = sb("hmS", [128, 4, 512], F32, nsub=4)
        stt = sb("stt", [128, 4, 256], F32, nsub=4)
        Cbf = sb("Cbf", [128, 4, 256], BF16, nsub=4)
        qsb = sb("qsb", [128, 4, 128], BF16, nsub=4)
        AT = sb("AT", [128, 4, 128], BF16, nsub=4)
        hb = [sb(f"hb{i}", [128, 1024], BF16) for i in range(3)]
        gate = sb("gate", [128, 4, 8], F32, nsub=4)
        lf = sb("lf", [128, 4, 4], F32, nsub=4)
        sm = sb("sm", [128, 64], F32)
        stat = [sb(f"stat{i}", [128, 8], F32) for i in range(4)]
        lam = sb("lam", [128, 4], F32)
        PS = [psb(f"ps{i}", [128, 512], F32) for i in range(8)]
        PSbf = [Buf(PS[i].t.bitcast(BF16) if hasattr(PS[i].t, "bitcast") else None, f"psbf{i}") for i in range(8)]
        for i in range(8):
            PS[i].trk[0].excl = True
            PSbf[i].trk = PS[i].trk

        ring_state = {"a": 0, "hb": 0, "f": 0}

        def ringA():
            i = ring_state["a"]
            ring_state["a"] = (i + 1) % 4
            return i

        def act(out, in_, func, r, w, bias=None, scale=None, accum_out=None):
            kw = {}
            if bias is not None:
                kw["bias"] = bias
            if scale is not None:
                kw["scale"] = scale
            if accum_out is not None:
                kw["accum_out"] = accum_out
            P.op("act", lambda e: e.activation(out=out, in_=in_, func=func, **kw), r=r, w=w)

        def mm(out, lhsT, rhs, start, stop, r, w):
            P.op("pe", lambda e: e.matmul(out, lhsT=lhsT, rhs=rhs, start=start, stop=stop), r=r, w=w)

        def tt(eng, out, in0, in1, op, r, w):
            P.op(eng, lambda e: e.tensor_tensor(out=out, in0=in0, in1=in1, op=op), r=r, w=w)

        def ts(eng, out, in0, s1, op0, r, w, s2=None, op1=None):
            if op1 is None:
                P.op(eng, lambda e: e.tensor_scalar(out=out, in0=in0, scalar1=s1, scalar2=None, op0=op0), r=r, w=w)
            else:
                P.op(eng, lambda e: e.tensor_scalar(out=out, in0=in0, scalar1=s1, scalar2=s2, op0=op0, op1=op1), r=r, w=w)

        def stt_(out, in0, scalar, in1, op0, op1, r, w):
            P.op("dve", lambda e: e.scalar_tensor_tensor(out=out, in0=in0, scalar=scalar, in1=in1, op0=op0, op1=op1), r=r, w=w)

        def cp(eng, out, in_, r, w):
            if eng == "act":
                P.op("act", lambda e: e.activation(out=out, in_=in_, func=AF.Copy), r=r, w=w)
            else:
                P.op(eng, lambda e: e.tensor_copy(out=out, in_=in_), r=r, w=w)

        lv = PR["lamv"]
        tt("dve", F[0].t[:, 0:64], lv.t[:, 0:64], lv.t[:, 64:128], ALU.mult, r=[lv], w=[F[0]])
        P.op("dve", lambda e: e.tensor_reduce(out=lam.t[:, 0:1], in_=F[0].t[:, 0:64], axis=AX.X, op=ALU.add), r=[F[0]], w=[lam])
        tt("dve", F[0].t[:, 0:64], lv.t[:, 128:192], lv.t[:, 192:256], ALU.mult, r=[lv], w=[F[0]])
        P.op("dve", lambda e: e.tensor_reduce(out=lam.t[:, 1:2], in_=F[0].t[:, 0:64], axis=AX.X, op=ALU.add), r=[F[0]], w=[lam])
        act(lam.t[:, 0:2], lam.t[:, 0:2], AF.Exp, r=[lam], w=[lam])
        tt("dve", lam.t[:, 2:3], lam.t[:, 1:2], lam.t[:, 0:1], ALU.subtract, r=[lam], w=[lam])
        ts("dve", lam.t[:, 3:4], lam.t[:, 2:3], -LAM_INIT, ALU.add, r=[lam], w=[lam])
        gd08 = sb("gd08", [128, 1], F32)
        ts("dve", gd08.t[:], PR["gdiff"].t[:], 1.0 - LAM_INIT, ALU.mult, r=[PR["gdiff"]], w=[gd08])
        P.op("pool", lambda e: e.memset(mvb.t[:, :, :, 128:256], 1.0), w=[mvb])

        _dbg = {}
        def src_slab(i):
            if i < 6:
                return dram["w_in"].ap().rearrange("(k p) c -> p k c", p=128)[:, :, i * 512:(i + 1) * 512], "gpre"
            if i < 8:
                j = i - 6
                return dram["w_out"].ap().rearrange("(k p) c -> p k c", p=128)[:, :, j * 512:(j + 1) * 512], None
            j = i - 8
            g, q = j // 4, j % 4
            if q < 2:
                s_ = 2 * g + q
                return dram["w_up"].ap().rearrange("(k p) c -> p k c", p=128)[:, :, s_ * 512:(s_ + 1) * 512], "gmlp"
            cg = q - 2
            return dram["w_down"].ap().rearrange("(g k p) c -> p g k c", g=4, k=8, p=128)[:, g, :, cg * 512:(cg + 1) * 512], None

        stg = [y, xc, kT, vd]
        stg_ap = [y.t[:].rearrange("p a (b c) -> p (a b) c", b=2), xc.t[:].rearrange("p a (b c) -> p (a b) c", b=2),
                  kT.t.bitcast(F32)[:].rearrange("p a (b c) -> p (a b) c", b=2), vd.t.bitcast(F32)[:].rearrange("p (a b) c -> p a (b c)", b=2)]
        NSTG = 4
        cast_eng = ["dve", "pool"]

        def pro_load(i):
            src, gname = src_slab(i)
            sbuf_ = stg[i % NSTG]
            sap = stg_ap[i % NSTG]
            P.dma("sp", (lambda e, sap=sap, src=src: e.dma_start(out=sap, in_=src)), sbuf_[0], w=[sbuf_])

        for i in range(NSTG):
            pro_load(i)
        for i in range(24):
            src, gname = src_slab(i)
            sbuf_ = stg[i % NSTG]
            sap = stg_ap[i % NSTG]
            slot = wr[i % NW]
            if gname is None:
                for hh in range(2):
                    cp("pool" if hh == 0 else "dve", slot.t[:, hh * 4:(hh + 1) * 4, :], sap[:, hh * 4:(hh + 1) * 4, :], r=[sbuf_], w=[slot])
            else:
                for k in range(8):
                    if k % 2 == 0:
                        ts("dve", slot.t[:, k, :], sap[:, k, :], PR[gname].t[:, k:k + 1], ALU.mult, r=[sbuf_, PR[gname]], w=[slot])
                    else:
                        act(slot.t[:, k, :], sap[:, k, :], AF.Copy, r=[sbuf_, PR[gname]], w=[slot], scale=PR[gname].t[:, k:k + 1])
            P.dma("act", (lambda e, slot=slot, i=i: e.dma_start(out=dram["wbf"].ap()[i], in_=slot.t[:])), slot[0], r=[slot], w=[wbf_trk[i]])
            if i + NSTG < 24:
                pro_load(i + NSTG)
        gsrc = dram["w_in"].ap().rearrange("(k p) c -> p k c", p=128)[:, :, 3072:3080]
        P.dma("sp", lambda e: e.dma_start(out=wgs.t[:], in_=gsrc), wgs[0], w=[wgs])
        for k in range(8):
            ts("dve", wg.t[:, k, :], wgs.t[:, k, :], PR["gpre"].t[:, k:k + 1], ALU.mult, r=[wgs, PR["gpre"]], w=[wg])

        wstate = {"issued": 0, "cur": 0}
        ORDER = [2, 3, 0, 1, 4, 5, 6, 7] + [8, 9, 12, 13, 10, 11, 16, 17, 14, 15, 20, 21, 18, 19, 22, 23]
        total_slabs = 24 * nseq * NCH

        def w_issue():
            i = wstate["issued"]
            if i >= total_slabs:
                return
            slot = wr[i % NW]
            sl = ORDER[i % 24]
            P.dma("sp", (lambda e, slot=slot, sl=sl: e.dma_start(out=slot.t[:], in_=dram["wbf"].ap()[sl])), slot[0], r=[wbf_trk[sl]], w=[slot])
            wstate["issued"] += 1

        def w_get(expect):
            i = wstate["cur"]
            assert ORDER[i % 24] == expect, (i, expect)
            while wstate["issued"] <= i:
                w_issue()
            return wr[i % NW]

        def w_done():
            wstate["cur"] += 1
            while wstate["issued"] < min(wstate["cur"] + NW, total_slabs):
                w_issue()

        def rstd_from_ssq(stat_b, col, n, width):
            a = stat_b.t[:, col:col + n]
            act(a, a, AF.Ln, r=[stat_b], w=[stat_b], bias=C_eps.t[:, 0:1], scale=1.0 / width)
            act(a, a, AF.Exp, r=[stat_b], w=[stat_b], scale=-0.5)

        C_eps = sb("c_eps", [128, 2], F32)
        P.op("pool", lambda e: e.memset(C_eps.t[:, 0:1], EPS), w=[C_eps])
        P.op("pool", lambda e: e.memset(C_eps.t[:, 1:2], 1.0), w=[C_eps])
        eps_ap = C_eps.t[:, 0:1]
        one_ap = C_eps.t[:, 1:2]

        junk = sb("junk", [128, 1024], BF16)

        def norm_stats(t, stb, col):
            act(junk.t[:], xc.t[:, t, :], AF.Square, r=[xc[t]], w=[stb], accum_out=stb.t[:, col:col + 1])

        def norm_apply(t, stb, col):
            i = ring_state["hb"]
            ring_state["hb"] = (i + 1) % 3
            hbb = hb[i]
            ts("dve", hbb.t[:], xc.t[:, t, :], stb.t[:, col:col + 1], ALU.mult, r=[xc[t], stb], w=[hbb])
            return hbb

        def norm_transpose(t, hbb):
            pi = ringA()
            pt = PSbf[pi].t
            for k in range(8):
                P.op("pe", (lambda e, k=k: e.transpose(out=pt[:, k * 128:(k + 1) * 128], in_=hbb.t[:, k * 128:(k + 1) * 128], identity=C["ident_bf"].t[:])),
                     r=[hbb, C["ident_bf"]], w=[PS[pi]])
            cp("act", hT.t[:, :, t * 128:(t + 1) * 128], pt[:, 0:1024].rearrange("p (k c) -> p k c", k=8), r=[PS[pi]], w=[hT[t]])

        def norm_all_to_hT():
            stb = stat[0]
            for t in range(4):
                norm_stats(t, stb, t)
            rstd_from_ssq(stb, 0, 4, D)
            for t in range(4):
                hbb = norm_apply(t, stb, t)
                norm_transpose(t, hbb)

        def ynorm_residual(t, gname):
            stb = stat[2 + (t % 2)]
            ys = y.t[:, t, :]
            act(junk.t[:], ys, AF.Square, r=[y[t]], w=[stb], accum_out=stb.t[:, 0:1])
            rstd_from_ssq(stb, 0, 1, D)
            stt_(ys, ys, stb.t[:, 0:1], PR[gname].t[:], ALU.mult, ALU.mult, r=[y[t], stb, PR[gname]], w=[y[t]])
            tt("pool" if t % 2 else "dve", xc.t[:, t, :], xc.t[:, t, :], ys, ALU.add, r=[xc[t], y[t]], w=[xc[t]])

        def chunk(b, c):
            tok0 = b * S + c * CH
            first = (c == 0)
            for t in range(4):
                P.dma("sp", (lambda e, t=t: e.dma_start(out=xc.t[:, t, :], in_=dram["x"].ap()[tok0 + t * 128: tok0 + (t + 1) * 128, :])), xc[t], w=[xc[t]])
            P.dma("sp", lambda e: e.dma_start(out=cs.t[:, 0, :], in_=dram["cos_t"].ap()[:, c * 512:(c + 1) * 512]), cs[0], w=[cs])
            P.dma("sp", lambda e: e.dma_start(out=cs.t[:, 1, :], in_=dram["sin_t"].ap()[:, c * 512:(c + 1) * 512]), cs[0], w=[cs])
            _stage(1)
            norm_all_to_hT()
            _stage(2)

            def modeB(slab, j):
                pi = ringA()
                for k in range(8):
                    mm(PS[pi].t[:, :], slab.t[:, k, j * 128:(j + 1) * 128], hT.t[:, k, :], k == 0, k == 7, r=[slab, hT], w=[PS[pi]])
                return pi

            def rope(pi, dst_ap, dst_trk, par):
                xbb = ET[par]
                cp("act", xbb.t[:], PS[pi].t[:, :], r=[PS[pi]], w=[xbb])
                _stage(2111)
                p2 = ringA()
                mm(PS[p2].t[:, :], C["perm_bf"].t[:], xbb.t[:], True, True, r=[C["perm_bf"], xbb], w=[PS[p2]])
                _stage(2112)
                t1, t2 = F[2 * par], F[2 * par + 1]
                tt("dve", t1.t[:, 0:512], PS[pi].t[:, :], cs.t[:, 0, :], ALU.mult, r=[PS[pi], cs], w=[t1])
                _stage(2113)
                tt("dve", t2.t[:, 0:512], PS[p2].t[:, :], cs.t[:, 1, :], ALU.mult, r=[PS[p2], cs], w=[t2])
                _stage(2114)
                tt(ROPE_ADD_ENG, dst_ap, t1.t[:, 0:512], t2.t[:, 0:512], ALU.add, r=[t1, t2], w=dst_trk)

            _stage(22)
            slab = w_get(2)
            for t in range(4):
                pi = ringA()
                for k in range(8):
                    mm(PS[pi].t[:, :], hT.t[:, k, t * 128:(t + 1) * 128], slab.t[:, k, :], k == 0, k == 7, r=[hT[t], slab], w=[PS[pi]])
                cp("dve", vd.t[:, c * 4 + t, :], PS[pi].t[:, :], r=[PS[pi]], w=[vd[c * 4 + t]])
            w_done()
            _stage(23)
            slab = w_get(3)
            for j in range(4):
                pi = modeB(slab, j)
                raw, acc, sg = F[4], F[5], F[6]
                if first:
                    P.op("pool", lambda e: e.memset(raw.t[:, 0:3], 0.0), w=[raw])
                else:
                    cp("pool", raw.t[:, 0:3], hal.t[:, j, 0:3], r=[hal[j]], w=[raw])
                cp("act", raw.t[:, 3:515], PS[pi].t[:, :], r=[PS[pi]], w=[raw])
                cp("pool", hal.t[:, j, 0:3], raw.t[:, 512:515], r=[raw], w=[hal[j]])
                cw = PR["convw"]
                ts("dve", acc.t[:, 0:512], raw.t[:, 3:515], cw.t[:, j * 4 + 3:j * 4 + 4], ALU.mult, r=[raw, cw, PR["convb"]], w=[acc],
                   s2=PR["convb"].t[:, j:j + 1], op1=ALU.add)
                for tap in (2, 1, 0):
                    stt_(acc.t[:, 0:512], raw.t[:, tap:tap + 512], cw.t[:, j * 4 + tap:j * 4 + tap + 1], acc.t[:, 0:512], ALU.mult, ALU.add,
                         r=[raw, cw, acc], w=[acc])
                act(sg.t[:, 0:512], acc.t[:, 0:512], AF.Exp, r=[acc], w=[sg], scale=-1.0)
                act(sg.t[:, 0:512], sg.t[:, 0:512], AF.Ln, r=[sg], w=[sg], bias=one_ap, scale=1.0)
                act(sg.t[:, 0:512], sg.t[:, 0:512], AF.Exp, r=[sg], w=[sg], scale=-1.0)
                if j < 2:
                    stt_(mqT.t[:, j, :], acc.t[:, 0:512], 0.125, sg.t[:, 0:512], ALU.mult, ALU.mult, r=[acc, sg], w=[mqT[j]])
                else:
                    tt("dve", mkT.t[:, j - 2, :], acc.t[:, 0:512], sg.t[:, 0:512], ALU.mult, r=[acc, sg], w=[mkT[j - 2]])
            w_done()
            slab = w_get(0)
            _stage(210)
            for h in range(4):
                pi = modeB(slab, h)
                _stage(211)
                rope(pi, qT.t[:, h, :], [qT[h]], h % 2)
                _stage(212)
            w_done()
            _stage(21)
            slab = w_get(1)
            for h in range(4):
                pi = modeB(slab, h)
                rope(pi, kT.t[:, h, c * 512:(c + 1) * 512], [kT[h * 4 + c]], h % 2)
            w_done()
            _stage(24)
            slab = w_get(4)
            for t in range(4):
                pi = ringA()
                for k in range(8):
                    mm(PS[pi].t[:, :], hT.t[:, k, t * 128:(t + 1) * 128], slab.t[:, k, :], k == 0, k == 7, r=[hT[t], slab], w=[PS[pi]])
                cp("dve", mvb.t[:, t, :, 0:128], PS[pi].t[:, :].rearrange("p (h v) -> p h v", h=4), r=[PS[pi]], w=[mvb[t]])
                pg = ringA()
                for k in range(8):
                    mm(PS[pg].t[:, 0:8], hT.t[:, k, t * 128:(t + 1) * 128], wg.t[:, k, :], k == 0, k == 7, r=[hT[t], wg], w=[PS[pg]])
                tt("dve", gate.t[:, t, :], PS[pg].t[:, 0:8], PR["gbias"].t[:], ALU.add, r=[PS[pg], PR["gbias"]], w=[gate[t]])
                act(lf.t[:, t, :], gate.t[:, t, 4:8], AF.Exp, r=[gate[t]], w=[lf[t]], scale=-1.0)
                act(lf.t[:, t, :], lf.t[:, t, :], AF.Ln, r=[lf[t]], w=[lf[t]], bias=one_ap, scale=1.0)
                ts("dve", lf.t[:, t, :], lf.t[:, t, :], -1.0, ALU.mult, r=[lf[t]], w=[lf[t]])
            w_done()
            slab_mo = w_get(5)
            _stage(3)

            for h in range(4):
                mlstm_prep(b, c, h)
                fin = attention(b, c, h, mid=(lambda h=h: mlstm_prep2(b, c, h)))
                _stage(4)
                mlstm_tile(b, c, h)
                fin()
                _stage(5)
            mlstm_post_all(slab_mo)
            w_done()

            _stage(6)
            s0 = w_get(6)
            assert wstate["issued"] > wstate["cur"] + 1
            s1 = wr[(wstate["cur"] + 1) % NW]
            pend = []
            for t in range(4):
                for cg, sl_ in ((0, s0), (1, s1)):
                    pi = ringA()
                    for k in range(8):
                        mm(PS[pi].t[:, :], oT.t[:, k, t * 128:(t + 1) * 128], sl_.t[:, k, :], k == 0, k == 7, r=[oT, sl_], w=[PS[pi]])
                    cp("act" if cg == 0 else "dve", y.t[:, t, cg * 512:(cg + 1) * 512], PS[pi].t[:, :], r=[PS[pi]], w=[y[t]])
                if len(pend) == 2:
                    norm_transpose(*pend.pop(0))
                ynorm_residual(t, "gpost_b")
                stb = stat[t % 2]
                norm_stats(t, stb, 4)
                rstd_from_ssq(stb, 4, 1, D)
                hbb = norm_apply(t, stb, 4)
                pend.append((t, hbb))
            w_done()
            assert w_get(7) is s1
            w_done()
            for pp in pend:
                norm_transpose(*pp)
            _stage(7)
            def mlp_up(g):
                uT = uTb[g % 2]
                for s_ in range(2):
                    slab = w_get(8 + g * 4 + s_)
                    for j in range(4):
                        pi = modeB(slab, j)
                        rr = F[(s_ * 4 + j) % 4]
                        act(rr.t[:, 0:512], PS[pi].t[:, :], AF.Relu, r=[PS[pi]], w=[rr])
                        tt("pool" if j % 2 else "dve", uT.t[:, s_ * 4 + j, :], rr.t[:, 0:512], rr.t[:, 0:512], ALU.mult, r=[rr], w=[uT[s_ * 4 + j]])
                    w_done()

            def mlp_down(g):
                uT = uTb[g % 2]
                for cg in range(2):
                    slab = w_get(8 + g * 4 + 2 + cg)
                    for t in range(4):
                        pi = ringA()
                        for k in range(8):
                            mm(PS[pi].t[:, :], uT.t[:, k, t * 128:(t + 1) * 128], slab.t[:, k, :], k == 0, k == 7, r=[uT, slab], w=[PS[pi]])
                        ydst = y.t[:, t, cg * 512:(cg + 1) * 512]
                        if g == 0:
                            cp("act", ydst, PS[pi].t[:, :], r=[PS[pi]], w=[y[t]])
                        else:
                            tt("dve", ydst, ydst, PS[pi].t[:, :], ALU.add, r=[y[t], PS[pi]], w=[y[t]])
                    w_done()

            mlp_up(0)
            mlp_up(1)
            mlp_down(0)
            mlp_up(2)
            mlp_down(1)
            mlp_up(3)
            mlp_down(2)
            mlp_down(3)
            for t in range(4):
                ynorm_residual(t, "gmlppost_b")
                P.dma("sp", (lambda e, t=t: e.dma_start(out=dram["out"].ap()[tok0 + t * 128: tok0 + (t + 1) * 128, :], in_=xc.t[:, t, :])),
                      xc[t], r=[xc[t]], store=True)

        def attention(b, c, h, mid=None):
            nkb = 4 * c + 4
            ON = [PS[4], PS[5]]
            DEN = [PS[6], PS[7]]
            ktr = [kT[h * 4 + cc] for cc in range(c + 1)]
            et_state = {"i": 0}

            def qk(kb):
                qlo = max(0, kb - 4 * c) * 128
                n = 512 - qlo
                res = []
                for m in range(2):
                    pi = ringA()
                    mm(PS[pi].t[:, 0:n], kT.t[m * 64:(m + 1) * 64, h, kb * 128:(kb + 1) * 128], qT.t[m * 64:(m + 1) * 64, h, qlo:512], True, True,
                       r=[kT[h * 4 + kb // 4], qT[h]], w=[PS[pi]])
                    ei = et_state["i"]
                    et_state["i"] = (ei + 1) % 4
                    et = ET[ei]
                    act(et.t[:, 0:n], PS[pi].t[:, 0:n], AF.Exp, r=[PS[pi]], w=[et], scale=0.125)
                    if kb >= 4 * c:
                        tt("pool", et.t[:, 0:128], et.t[:, 0:128], C["mask_bf"].t[:], ALU.mult, r=[et, C["mask_bf"]], w=[et])
                    res.append((et, qlo, n))
                return res

            def pv(kb, res):
                for m in range(2):
                    et, qlo, n = res[m]
                    mm(ON[m].t[:, qlo:512], vd.t[:, kb, h * 128:(h + 1) * 128], et.t[:, 0:n], kb == 0, kb == nkb - 1, r=[vd[kb], et], w=[ON[m]])
                    mm(DEN[m].t[:, qlo:512], C["ones_bf"].t[:], et.t[:, 0:n], kb == 0, kb == nkb - 1, r=[C["ones_bf"], et], w=[DEN[m]])

            prev = qk(0)
            for kb in range(nkb):
                nxt = qk(kb + 1) if kb + 1 < nkb else None
                pv(kb, prev)
                prev = nxt
                if kb == 0 and mid is not None:
                    mid()
            T1, T2 = F[0], F[1]
            a1, a2 = T1.t[:, 0:512], T2.t[:, 0:512]
            act(a1, DEN[0].t[:, :], AF.Ln, r=[DEN[0]], w=[T1])
            act(a1, a1, AF.Exp, r=[T1], w=[T1], scale=-1.0)
            act(a2, DEN[1].t[:, :], AF.Ln, r=[DEN[1]], w=[T2])
            act(a2, a2, AF.Exp, r=[T2], w=[T2], scale=-1.0)
            tt("dve", a1, ON[0].t[:, :], a1, ALU.mult, r=[ON[0], T1], w=[T1])
            tt("dve", a2, ON[1].t[:, :], a2, ALU.mult, r=[ON[1], T2], w=[T2])
            stt_(a1, a2, lam.t[:, 3:4], a1, ALU.mult, ALU.add, r=[T1, T2, lam], w=[T1])
            tt("dve", a2, a1, a1, ALU.mult, r=[T1], w=[T2])

            def finish():
                pi = ringA()
                mm(PS[pi].t[:, :], C["ones_f"].t[:], a2, True, True, r=[C["ones_f"], T2], w=[PS[pi]])
                act(a2, PS[pi].t[:, :], AF.Ln, r=[PS[pi]], w=[T2], bias=eps_ap, scale=1.0 / 128)
                act(a2, a2, AF.Exp, r=[T2], w=[T2], scale=-0.5)
                stt_(oT.t[:, h, :], a1, gd08.t[:, 0:1], a2, ALU.mult, ALU.mult, r=[T1, T2, gd08], w=[oT[h]])
            return finish

        def mlstm_prep(b, c, t):
            first = (c == 0 and t == 0)
            tc_ = slice(t * 128, (t + 1) * 128)
            if first:
                P.op("pool", lambda e: e.memset(stt.t[:], 0.0), w=[stt])
                P.op("pool", lambda e: e.memset(Cbf.t[:], 0.0), w=[Cbf])
            lft = lf.t[:, t, :]
            pg = ringA()
            mm(PS[pg].t[:, 0:4], C["triu_f"].t[:], lft, True, True, r=[C["triu_f"], lf[t]], w=[PS[pg]])
            mm(PS[pg].t[:, 4:8], C["ones_f"].t[:], lft, True, True, r=[C["ones_f"], lf[t]], w=[PS[pg]])
            bc = sm.t[:, 0:4]
            wk = sm.t[:, 4:8]
            eG = sm.t[:, 8:12]
            tt("dve", bc, gate.t[:, t, 0:4], PS[pg].t[:, 0:4], ALU.subtract, r=[gate[t], PS[pg]], w=[sm])
            tt("dve", wk, PS[pg].t[:, 4:8], bc, ALU.add, r=[PS[pg], sm], w=[sm])
            act(wk, wk, AF.Exp, r=[sm], w=[sm])
            act(eG, PS[pg].t[:, 4:8], AF.Exp, r=[PS[pg]], w=[sm])
            RL, EB, DT, DM = F[2], F[3], F[4], F[5]
            for h in range(4):
                ts("dve", RL.t[:, h * 128:(h + 1) * 128], C["triu_f"].t[:], lf.t[:, t, h:h + 1], ALU.mult, r=[C["triu_f"], lf[t]], w=[RL])

        def mlstm_prep2(b, c, t):
            tc_ = slice(t * 128, (t + 1) * 128)
            RL, EB, DT, DM = F[2], F[3], F[4], F[5]
            pB = ringA()
            mm(PS[pB].t[:, :], C["ones_f"].t[:], RL.t[:, 0:512], True, True, r=[C["ones_f"], RL], w=[PS[pB]])
            act(EB.t[:, 0:512], PS[pB].t[:, :], AF.Exp, r=[PS[pB]], w=[EB])
            pBm = ringA()
            mm(PS[pBm].t[:, :], C["ones_f"].t[:], RL.t[:, 0:512], True, False, r=[C["ones_f"], RL], w=[PS[pBm]])
            mm(PS[pBm].t[:, :], C["ident_f"].t[:], C["maskneg4"].t[:], False, True, r=[C["ident_f"], C["maskneg4"]], w=[PS[pBm]])
            for h in range(4):
                hs = slice(h * 128, (h + 1) * 128)
                act(DT.t[:, hs], PS[pBm].t[:, hs], AF.Exp, r=[PS[pBm], sm], w=[DT], bias=sm.t[:, h:h + 1], scale=1.0)
            pT = ringA()
            for j in range(2):
                P.op("pe", (lambda e, j=j: e.transpose(out=PSbf[pT].t[:, j * 128:(j + 1) * 128], in_=mkT.t[:, j, tc_], identity=C["ident_bf"].t[:])),
                     r=[mkT[j], C["ident_bf"]], w=[PS[pT]])
            for h in range(4):
                ts("dve", ktil.t[:, h * 64:(h + 1) * 64], PSbf[pT].t[:, h * 64:(h + 1) * 64], sm.t[:, 4 + h:5 + h], ALU.mult, r=[PS[pT], sm], w=[ktil])
            for h in range(4):
                base = (h % 2) * 64
                j = h // 2
                ps_ = slice(base, base + 64)
                hs = slice(h * 128, (h + 1) * 128)
                tt("dve", qsb.t[ps_, h, :], mqT.t[ps_, j, tc_], EB.t[ps_, hs], ALU.mult, r=[mqT[j], EB], w=[qsb[h]])
            pSb = [ringA(), ringA()]
            for h in range(4):
                base = (h % 2) * 64
                j = h // 2
                ps_ = slice(base, base + 64)
                hs = slice(h * 128, (h + 1) * 128)
                pb_ = pSb[h % 2]
                mm(PS[pb_].t[:, hs], mkT.t[ps_, j, tc_], mqT.t[ps_, j, tc_], True, True, r=[mkT[j], mqT[j]], w=[PS[pb_]])
            for h in range(4):
                hs = slice(h * 128, (h + 1) * 128)
                pb_ = pSb[h % 2]
                tt("dve", AT.t[:, h, :], PS[pb_].t[:, hs], DT.t[:, hs], ALU.mult, r=[PS[pb_], DT], w=[AT[h]])

        def mlstm_tile(b, c, t):
            tc_ = slice(t * 128, (t + 1) * 128)
            RL, EB, DT, DM = F[2], F[3], F[4], F[5]
            pN, pD, pS = 4, 5, 6
            HB = [((h % 2) * 64, h // 2, slice((h % 2) * 64, (h % 2) * 64 + 64), slice(h * 128, (h + 1) * 128)) for h in range(4)]
            pUs = []
            for h in range(4):
                base, j, ps_, hs = HB[h]
                mm(PS[pN].t[:, hs], mvb.t[:, t, h, 0:128], AT.t[:, h, :], True, False, r=[mvb[t], AT[h]], w=[PS[pN]])
                mm(PS[pN].t[:, hs], Cbf.t[ps_, h, 0:128], qsb.t[ps_, h, :], False, True, r=[Cbf[h], qsb[h]], w=[PS[pN]])
                mm(PS[pD].t[:, hs], C["ones_bf"].t[:], AT.t[:, h, :], True, False, r=[C["ones_bf"], AT[h]], w=[PS[pD]])
                mm(PS[pD].t[:, hs], Cbf.t[ps_, h, 128:256], qsb.t[ps_, h, :], False, True, r=[Cbf[h], qsb[h]], w=[PS[pD]])
                pU = ringA()
                pUs.append(pU)
                mm(PS[pU].t[:, 0:256], ktil.t[:, j * 128:(j + 1) * 128], mvb.t[:, t, h, :], True, True, r=[ktil, mvb[t]], w=[PS[pU]])
            for h in range(4):
                base, j, ps_, hs = HB[h]
                pU = pUs[h]
                stt_(stt.t[ps_, h, :], stt.t[ps_, h, :], sm.t[ps_, 8 + h:9 + h], PS[pU].t[ps_, 0:256], ALU.mult, ALU.add,
                     r=[stt[h], sm, PS[pU]], w=[stt[h]])
                cp("pool", Cbf.t[ps_, h, :], stt.t[ps_, h, :], r=[stt[h]], w=[Cbf[h]])
            act(DM.t[:, 0:512], PS[pD].t[:, :], AF.Abs, r=[PS[pD]], w=[DM])
            ts("dve", DM.t[:, 0:512], DM.t[:, 0:512], 1.0, ALU.max, r=[DM], w=[DM])
            act(DM.t[:, 0:512], DM.t[:, 0:512], AF.Ln, r=[DM], w=[DM])
            act(DM.t[:, 0:512], DM.t[:, 0:512], AF.Exp, r=[DM], w=[DM], scale=-1.0)
            tt("dve", hmS.t[:, :, tc_], PS[pN].t[:, :].rearrange("p (h v) -> p h v", h=4), DM.t[:, 0:512].rearrange("p (h v) -> p h v", h=4), ALU.mult,
               r=[PS[pN], DM], w=[hmS])

        def mlstm_post_all(slab_mo):
            SQ = [F[0], F[1], F[6], F[7]]
            SG = [F[2], F[3], F[4], F[5]]
            for h in range(4):
                hm = hmS.t[:, h, :]
                tt("pool" if h % 2 else "dve", SQ[h].t[:, 0:512], hm, hm, ALU.mult, r=[hmS[h]], w=[SQ[h]])
            for h in range(4):
                sq = SQ[h].t[:, 0:512]
                pi = ringA()
                mm(PS[pi].t[:, :], C["ones_f"].t[:], sq, True, True, r=[C["ones_f"], SQ[h]], w=[PS[pi]])
                act(sq, PS[pi].t[:, :], AF.Ln, r=[PS[pi]], w=[SQ[h]], bias=eps_ap, scale=1.0 / 128)
                act(sq, sq, AF.Exp, r=[SQ[h]], w=[SQ[h]], scale=-0.5)
                stt_(sq, hmS.t[:, h, :], PR["gml"].t[:, h:h + 1], sq, ALU.mult, ALU.mult, r=[hmS[h], PR["gml"], SQ[h]], w=[SQ[h]])
            for h in range(4):
                pm = ringA()
                for k in range(8):
                    mm(PS[pm].t[:, :], slab_mo.t[:, k, h * 128:(h + 1) * 128], hT.t[:, k, :], k == 0, k == 7, r=[slab_mo, hT], w=[PS[pm]])
                sg = SG[h].t[:, 0:512]
                act(sg, PS[pm].t[:, :], AF.Exp, r=[PS[pm]], w=[SG[h]], scale=-1.0)
                act(sg, sg, AF.Ln, r=[SG[h]], w=[SG[h]], bias=one_ap, scale=1.0)
                act(sg, sg, AF.Exp, r=[SG[h]], w=[SG[h]], scale=-1.0)
                tt("pool" if h % 2 else "dve", oT.t[:, 4 + h, :], SQ[h].t[:, 0:512], sg, ALU.mult, r=[SQ[h], SG[h]], w=[oT[4 + h]])

        try:
            for b in range(nseq):
                for c in range(NCH):
                    chunk(b, c)
        except _Stop:
            pass

        P.emit(st)
    return nc


_CACHE = {}


def _run(inputs, n_cores, nseq):
    consts = _consts()
    params = _params(inputs)
    x = np.ascontiguousarray(np.asarray(inputs["x"], dtype=np.float32))
    B = x.shape[0]
    assert B == n_cores * nseq
    key = (nseq,)
    if key not in _CACHE:
        _CACHE[key] = build(nseq)
    nc = _CACHE[key]
    shared = {
        "w_in": np.ascontiguousarray(np.asarray(inputs["w_in"][0], dtype=np.float32)),
        "w_out": np.ascontiguousarray(np.asarray(inputs["w_out"][0], dtype=np.float32)),
        "w_up": np.ascontiguousarray(np.asarray(inputs["w_up"][0], dtype=np.float32)),
        "w_down": np.ascontiguousarray(np.asarray(inputs["w_down"][0], dtype=np.float32)),
    }
    shared.update(consts)
    shared.update(params)
    in_maps = []
    for i in range(n_cores):
        m = dict(shared)
        m["x"] = x[i * nseq:(i + 1) * nseq].reshape(nseq * S, D)
        in_maps.append(m)
    res = run_bass_kernel_spmd(nc, in_maps, core_ids=list(range(n_cores)))
    outs = [np.asarray(r["out"]).reshape(nseq, S, D) for r in res.results]
    return np.concatenate(outs, axis=0).astype(np.float32)


def kernel(**inputs):
    return _run(inputs, 8, 4)
```
